# Optimizing a Trainium2 kernel written in Bass

```python
import math
import jax, jax.numpy as jnp
from jax import lax
import numpy as np

D_MODEL = 2048
BATCH = 4
SEQ = 2048
DEPTH = 4
DEC_BATCH = 128
DEC_SEQ = 1
PAST_LEN = 16384
PAGE_SIZE = 128

RET_WIDTH = D_MODEL // 2
RET_HEADS = 8
RET_HEAD_DIM = RET_WIDTH // RET_HEADS
RET_CHUNK = 128
ROPE_BASE = 10000.0
RWKV_WIDTH = D_MODEL - RET_WIDTH
RWKV_HEAD_DIM = 64
RWKV_HEADS = RWKV_WIDTH // RWKV_HEAD_DIM


def _lora_rank(mult, power, width):
    return max(32, int(round(mult * width ** power / 32)) * 32)


R_DECAY = _lora_rank(1.8, 0.5, RWKV_WIDTH)
R_AAA = _lora_rank(1.8, 0.5, RWKV_WIDTH)
R_MV = _lora_rank(1.3, 0.5, RWKV_WIDTH)
R_GATE = _lora_rank(0.6, 0.8, RWKV_WIDTH)
D_FF = ((8 * D_MODEL // 3 + 255) // 256) * 256

RET_COLS = 4 * RET_WIDTH
RWKV_COLS = 3 * RWKV_WIDTH + R_DECAY + R_AAA + R_GATE
IN_COLS = RET_COLS + RWKV_COLS
NORM_EPS = 1e-6
RET_LN_EPS = 1e-5
RWKV_LN_EPS = 64e-5

kernel_name = "hybrid_retention_rwkv7_step"


def rms_norm(x, g):
    xf = x.astype(jnp.float32)
    y = xf * lax.rsqrt(jnp.mean(xf * xf, axis=-1, keepdims=True) + NORM_EPS)
    return (y * g.astype(jnp.float32)).astype(x.dtype)


def head_norm(x, n_heads, eps):
    xf = x.astype(jnp.float32).reshape(x.shape[:-1] + (n_heads, -1))
    mu = jnp.mean(xf, axis=-1, keepdims=True)
    xc = xf - mu
    var = jnp.mean(xc * xc, axis=-1, keepdims=True)
    return (xc * lax.rsqrt(var + eps)).reshape(x.shape)


def rope(x, pos):
    half = x.shape[-1] // 2
    inv_freq = ROPE_BASE ** (-jnp.arange(half, dtype=jnp.float32) / half)
    ang = pos[:, None] * inv_freq[None, :]
    cos = jnp.cos(ang)[None, :, None, :]
    sin = jnp.sin(ang)[None, :, None, :]
    x1, x2 = x[..., :half], x[..., half:]
    return jnp.concatenate([x1 * cos - x2 * sin, x1 * sin + x2 * cos], axis=-1)


def retention_chunkwise(q, k, v, s0, log_gamma):
    b, t, h, d = q.shape
    c = t if t <= RET_CHUNK else math.gcd(t, RET_CHUNK)
    n = t // c
    q, k, v = (z.reshape(b, n, c, h, d) for z in (q, k, v))
    idx = jnp.arange(c, dtype=jnp.float32)
    diff = idx[:, None] - idx[None, :]
    decay_in = jnp.where(diff >= 0, jnp.exp(log_gamma[:, None, None] * jnp.maximum(diff, 0.0)), 0.0)
    scores = jnp.einsum('bnihd,bnjhd->bnhij', q, k) * decay_in
    y_intra = jnp.einsum('bnhij,bnjhe->bnihe', scores, v)
    k_tail = k * jnp.exp((c - 1 - idx)[:, None] * log_gamma[None, :])[:, :, None]
    kv_chunks = jnp.einsum('bnjhd,bnjhe->nbhde', k_tail, v)
    chunk_decay = jnp.exp(c * log_gamma)[None, :, None, None]

    def step(s, kv):
        return s * chunk_decay + kv, s

    s_final, s_before = lax.scan(step, s0, kv_chunks)
    q_head = q * jnp.exp((idx + 1)[:, None] * log_gamma[None, :])[:, :, None]
    y_inter = jnp.einsum('bnihd,nbhde->bnihe', q_head, s_before)
    return (y_intra + y_inter).reshape(b, t, h, d), s_final


def retention_mixer(p_ret, state, pos, ln_w, log_gamma):
    b, t, _ = p_ret.shape
    q, k, v, g = jnp.split(p_ret.astype(jnp.float32), 4, axis=-1)
    shp = (b, t, RET_HEADS, RET_HEAD_DIM)
    q = rope(q.reshape(shp), pos)
    k = rope(k.reshape(shp), pos) * (RET_HEAD_DIM ** -0.5)
    y, s_new = retention_chunkwise(q, k, v.reshape(shp), state.astype(jnp.float32), log_gamma)
    y = head_norm(y.reshape(b, t, RET_WIDTH), RET_HEADS, RET_LN_EPS) * ln_w.astype(jnp.float32)
    return (jax.nn.silu(g) * y).astype(p_ret.dtype), s_new


def rwkv7_scan(r, decay, k, v, kk, a, s0):
    def step(s, inp):
        r_t, w_t, k_t, v_t, kk_t, a_t = inp
        sa = jnp.einsum('bhvk,bhk->bhv', s, -kk_t)
        s = (s * w_t[:, :, None, :] + sa[..., None] * (kk_t * a_t)[:, :, None, :]
             + v_t[..., None] * k_t[:, :, None, :])
        return s, jnp.einsum('bhvk,bhk->bhv', s, r_t)

    xs = tuple(jnp.moveaxis(z, 1, 0) for z in (r, decay, k, v, kk, a))
    s_final, out = lax.scan(step, s0, xs)
    return jnp.moveaxis(out, 0, 1), s_final


def rwkv7_mixer(p, state, v_first, w0, w_up, a0, a_up, g_up, k_k, k_a, r_k, ln_w, ln_b, v0, v_up):
    b, t, _ = p.shape
    f32 = jnp.float32
    pf = p.astype(f32)
    o1 = RWKV_WIDTH
    o2 = 2 * RWKV_WIDTH
    o3 = 3 * RWKV_WIDTH
    o4 = o3 + R_DECAY
    o5 = o4 + R_AAA
    o6 = o5 + R_GATE
    r, k, v = pf[..., :o1], pf[..., o1:o2], pf[..., o2:o3]
    xw, xa, xg = pf[..., o3:o4], pf[..., o4:o5], pf[..., o5:o6]
    w = -jax.nn.softplus(-(w0.astype(f32) + jnp.tanh(xw) @ w_up.astype(f32))) - 0.5
    decay = jnp.exp(-jnp.exp(w))
    a = jax.nn.sigmoid(a0.astype(f32) + xa @ a_up.astype(f32))
    g = jax.nn.sigmoid(xg) @ g_up.astype(f32)
    if v0 is None:
        v_first = v
    else:
        xv = pf[..., o6:]
        v = v + (v_first - v) * jax.nn.sigmoid(v0.astype(f32) + xv @ v_up.astype(f32))

    def heads(z):
        return z.reshape(b, t, RWKV_HEADS, RWKV_HEAD_DIM)

    kk = heads(k * k_k.astype(f32))
    kk = kk / jnp.maximum(jnp.sqrt(jnp.sum(kk * kk, axis=-1, keepdims=True)), 1e-12)
    k = k * (1.0 + (a - 1.0) * k_a.astype(f32))
    rh, kh, vh, ah, wh = heads(r), heads(k), heads(v), heads(a), heads(decay)
    out, s_new = rwkv7_scan(rh, wh, kh, vh, kk, ah, state.astype(f32))
    y = head_norm(out.reshape(b, t, RWKV_WIDTH), RWKV_HEADS, RWKV_LN_EPS) * ln_w.astype(f32) + ln_b.astype(f32)
    bonus = jnp.sum(rh * kh * r_k.astype(f32), axis=-1, keepdims=True) * vh
    y = (y + bonus.reshape(b, t, RWKV_WIDTH)) * g
    return y.astype(p.dtype), s_new, v_first


def trunk(x, pos, ret_state, rwkv_state, shift_state, wts):
    log_gamma = jnp.log1p(-jnp.exp2(-5.0 - jnp.arange(RET_HEADS, dtype=jnp.float32)))
    h = x
    v_first = None
    ret_out, rwkv_out, shift_out = [], [], []
    for l in range(DEPTH):
        xn = rms_norm(h, wts['g_attn'][l])
        if l == 0:
            w_mix, mu = wts['w_in'][0], wts['mu_shift'][0]
            v0, v_up = None, None
        else:
            w_mix = jnp.concatenate([wts['w_in'][l], wts['w_in_vres'][l - 1]], axis=-1)
            mu = jnp.concatenate([wts['mu_shift'][l], wts['mu_shift_vres'][l - 1]], axis=-1)
            v0, v_up = wts['rwkv_v0'][l - 1], wts['rwkv_v_up'][l - 1]
        proj = xn @ w_mix
        p_ret, p_rw = proj[..., :RET_COLS], proj[..., RET_COLS:]
        prev = shift_state[l].astype(xn.dtype) @ w_mix[:, RET_COLS:]
        p_prev = jnp.concatenate([prev[:, None, :], p_rw[:, :-1]], axis=1)
        p_rw = p_rw + (p_prev - p_rw) * mu
        y_ret, s_ret = retention_mixer(p_ret, ret_state[l], pos, wts['ret_ln_w'][l], log_gamma)
        y_rw, s_rw, v_first = rwkv7_mixer(
            p_rw, rwkv_state[l], v_first, wts['rwkv_w0'][l], wts['rwkv_w_up'][l], wts['rwkv_a0'][l],
            wts['rwkv_a_up'][l], wts['rwkv_g_up'][l], wts['rwkv_k_k'][l], wts['rwkv_k_a'][l],
            wts['rwkv_r_k'][l], wts['rwkv_ln_w'][l], wts['rwkv_ln_b'][l], v0, v_up)
        h = h + jnp.concatenate([y_ret, y_rw], axis=-1) @ wts['w_out'][l]
        hn = rms_norm(h, wts['g_ffn'][l])
        h = h + (jax.nn.silu(hn @ wts['w_gate'][l]) * (hn @ wts['w_up'][l])) @ wts['w_down'][l]
        ret_out.append(s_ret.astype(ret_state.dtype))
        rwkv_out.append(s_rw.astype(rwkv_state.dtype))
        shift_out.append(xn[:, -1, :].astype(shift_state.dtype))
    y = rms_norm(h, wts['g_final'])
    return y, jnp.stack(ret_out), jnp.stack(rwkv_out), jnp.stack(shift_out)


def setup_inputs(seed: int = 0) -> dict:
    key = jax.random.key(seed)
    ks = iter(jax.random.split(key, 40))
    f32 = jnp.float32

    def nrm(shape, scale):
        return jax.random.normal(next(ks), shape, f32) * scale

    def unif(shape, lo, hi):
        return jax.random.uniform(next(ks), shape, f32, minval=lo, maxval=hi)

    dm1 = DEPTH - 1
    return {
        "x_prompt": nrm((BATCH, SEQ, D_MODEL), 1.0),
        "x_sample": nrm((DEC_BATCH, DEC_SEQ, D_MODEL), 1.0),
        "state_ret": nrm((DEPTH, DEC_BATCH, RET_HEADS, RET_HEAD_DIM, RET_HEAD_DIM), 0.5),
        "state_rwkv": nrm((DEPTH, DEC_BATCH, RWKV_HEADS, RWKV_HEAD_DIM, RWKV_HEAD_DIM), 0.5),
        "state_shift": nrm((DEPTH, DEC_BATCH, D_MODEL), 1.0),
        "w_in": nrm((DEPTH, D_MODEL, IN_COLS), D_MODEL ** -0.5),
        "w_in_vres": nrm((dm1, D_MODEL, R_MV), D_MODEL ** -0.5),
        "mu_shift": unif((DEPTH, RWKV_COLS), 0.0, 1.0),
        "mu_shift_vres": unif((dm1, R_MV), 0.0, 1.0),
        "ret_ln_w": 1.0 + nrm((DEPTH, RET_WIDTH), 0.02),
        "rwkv_w0": unif((DEPTH, RWKV_WIDTH), -6.0, 0.0),
        "rwkv_w_up": nrm((DEPTH, R_DECAY, RWKV_WIDTH), 0.5 * R_DECAY ** -0.5),
        "rwkv_a0": nrm((DEPTH, RWKV_WIDTH), 0.1),
        "rwkv_a_up": nrm((DEPTH, R_AAA, RWKV_WIDTH), 0.5 * R_AAA ** -0.5),
        "rwkv_g_up": nrm((DEPTH, R_GATE, RWKV_WIDTH), R_GATE ** -0.5),
        "rwkv_v0": nrm((dm1, RWKV_WIDTH), 0.1),
        "rwkv_v_up": nrm((dm1, R_MV, RWKV_WIDTH), 0.5 * R_MV ** -0.5),
        "rwkv_k_k": 0.85 + nrm((DEPTH, RWKV_WIDTH), 0.02),
        "rwkv_k_a": 1.0 + nrm((DEPTH, RWKV_WIDTH), 0.02),
        "rwkv_r_k": nrm((DEPTH, RWKV_HEADS, RWKV_HEAD_DIM), 0.1),
        "rwkv_ln_w": 1.0 + nrm((DEPTH, RWKV_WIDTH), 0.02),
        "rwkv_ln_b": nrm((DEPTH, RWKV_WIDTH), 0.01),
        "w_out": nrm((DEPTH, D_MODEL, D_MODEL), D_MODEL ** -0.5),
        "g_attn": 1.0 + nrm((DEPTH, D_MODEL), 0.02),
        "g_ffn": 1.0 + nrm((DEPTH, D_MODEL), 0.02),
        "w_gate": nrm((DEPTH, D_MODEL, D_FF), D_MODEL ** -0.5),
        "w_up": nrm((DEPTH, D_MODEL, D_FF), D_MODEL ** -0.5),
        "w_down": nrm((DEPTH, D_FF, D_MODEL), D_FF ** -0.5),
        "g_final": 1.0 + nrm((D_MODEL,), 0.02),
    }


def reference(x_prompt, x_sample, state_ret, state_rwkv, state_shift, w_in, w_in_vres, mu_shift,
              mu_shift_vres, ret_ln_w, rwkv_w0, rwkv_w_up, rwkv_a0, rwkv_a_up, rwkv_g_up, rwkv_v0,
              rwkv_v_up, rwkv_k_k, rwkv_k_a, rwkv_r_k, rwkv_ln_w, rwkv_ln_b, w_out, g_attn, g_ffn,
              w_gate, w_up, w_down, g_final):
    wts = {
        'w_in': w_in, 'w_in_vres': w_in_vres, 'mu_shift': mu_shift, 'mu_shift_vres': mu_shift_vres,
        'ret_ln_w': ret_ln_w, 'rwkv_w0': rwkv_w0, 'rwkv_w_up': rwkv_w_up, 'rwkv_a0': rwkv_a0,
        'rwkv_a_up': rwkv_a_up, 'rwkv_g_up': rwkv_g_up, 'rwkv_v0': rwkv_v0, 'rwkv_v_up': rwkv_v_up,
        'rwkv_k_k': rwkv_k_k, 'rwkv_k_a': rwkv_k_a, 'rwkv_r_k': rwkv_r_k, 'rwkv_ln_w': rwkv_ln_w,
        'rwkv_ln_b': rwkv_ln_b, 'w_out': w_out, 'g_attn': g_attn, 'g_ffn': g_ffn,
        'w_gate': w_gate, 'w_up': w_up, 'w_down': w_down, 'g_final': g_final,
    }
    bp = x_prompt.shape[0]
    ret0 = jnp.zeros((DEPTH, bp) + state_ret.shape[2:], state_ret.dtype)
    rwkv0 = jnp.zeros((DEPTH, bp) + state_rwkv.shape[2:], state_rwkv.dtype)
    shift0 = jnp.zeros((DEPTH, bp, D_MODEL), state_shift.dtype)
    pos_prompt = jnp.arange(x_prompt.shape[1], dtype=jnp.float32)
    y_prompt, ret_p, rwkv_p, shift_p = trunk(x_prompt, pos_prompt, ret0, rwkv0, shift0, wts)
    pos_sample = PAST_LEN + jnp.arange(x_sample.shape[1], dtype=jnp.float32)
    y_sample, ret_s, rwkv_s, shift_s = trunk(x_sample, pos_sample, state_ret, state_rwkv, state_shift, wts)
    return (y_prompt, y_sample, ret_p, rwkv_p, shift_p, ret_s, rwkv_s, shift_s)
```

```python
import contextlib
import numpy as np
import concourse.bass as bass
import concourse.mybir as mybir
from concourse.bass_utils import run_bass_kernel_spmd

F32 = mybir.dt.float32
BF16 = mybir.dt.bfloat16
AF = mybir.ActivationFunctionType
ALU = mybir.AluOpType
AX = mybir.AxisListType

D = 2048
NKC = 16
DFF = 5632
NG = 11
RETW = 1024
SPC = 16
NSG = 2
NCORE = 4
SPCORE = SPC * NSG
TT = 128
NPAR = 104
C_DEC = float(np.exp(-0.5))
GAM = [1.0 - 2.0 ** (-5.0 - h) for h in range(8)]


class Sem:
    def __init__(self, h):
        self.h = h
        self.val = 0


class Eng:
    def __init__(self, name, sem):
        self.name = name
        self.sem = sem
        self.q = []
        self.seen = {}


class Tl:
    def __init__(self, t, name):
        self.t = t
        self.name = name
        self.w = None
        self.r = {}

    def __getitem__(self, k):
        return self.t[k]


class Sched:
    def __init__(self, nc, stack, ndma_sp=20, ndma_pool=8):
        self.nc = nc
        self.stack = stack
        self.E = {}
        for n in ("pe", "act", "dve", "pool", "sp"):
            self.E[n] = Eng(n, Sem(stack.enter_context(nc.semaphore("s_" + n))))
        self.dsem = {
            "sp": [Sem(stack.enter_context(nc.semaphore(f"dsp{i}"))) for i in range(ndma_sp)],
            "pool": [Sem(stack.enter_context(nc.semaphore(f"dpl{i}"))) for i in range(ndma_pool)],
        }
        self.dcnt = {"sp": 0, "pool": 0}
        self.nalloc = 0
        self.ps_rr = 0

    def sb(self, name, shape, dt=F32):
        t = self.stack.enter_context(self.nc.sbuf_tensor("sb_" + name, list(shape), dt))
        return Tl(t, name)

    def _wait(self, E, ev):
        sem, val = ev
        if E.seen.get(id(sem), 0) >= val:
            return
        E.seen[id(sem)] = val
        E.q.append(lambda e, h=sem.h, v=val: e.wait_ge(h, v))

    def _deps(self, E, reads, writes, selfsync):
        evs = []
        for b in reads:
            if b.w is not None:
                evs.append(b.w)
        for b in writes:
            if b.w is not None:
                evs.append(b.w)
            evs.extend(b.r.values())
        for ev in evs:
            if (not selfsync) and ev[0] is E.sem:
                continue
            self._wait(E, ev)

    def _reg(self, me, reads, writes):
        for b in reads:
            b.r[id(me[0])] = me
        for b in writes:
            b.w = me
            b.r = {}

    def op(self, en, fn, reads=(), writes=(), inc=True):
        E = self.E[en]
        self._deps(E, reads, writes, selfsync=(en != "pe"))
        if inc:
            E.sem.val += 1
            me = (E.sem, E.sem.val)
            E.q.append(lambda e, f=fn, h=E.sem.h: f(e).then_inc(h, 1))
        else:
            me = (E.sem, E.sem.val + 1)
            E.q.append(lambda e, f=fn: f(e))
        self._reg(me, reads, writes)

    def dma(self, qn, out, in_, reads=(), writes=()):
        E = self.E[qn]
        pool = self.dsem[qn]
        sem = pool[self.dcnt[qn] % len(pool)]
        self.dcnt[qn] += 1
        self._deps(E, reads, writes, selfsync=True)
        if sem.val > 0:
            self._wait(E, (sem, sem.val))
        sem.val += 16
        me = (sem, sem.val)
        E.q.append(lambda e, o=out, i=in_, h=sem.h: e.dma_start(out=o, in_=i).then_inc(h, 16))
        self._reg(me, reads, writes)

    def finish(self):
        E = self.E["sp"]
        for qn in ("sp", "pool"):
            for sem in self.dsem[qn]:
                if sem.val > 0:
                    self._wait(E, (sem, sem.val))
        for n in ("pe", "act", "dve", "pool"):
            s = self.E[n].sem
            if s.val > 0:
                self._wait(E, (s, s.val))


def _blk(w, cols):
    x = w[:, cols]
    return np.ascontiguousarray(x.reshape(NKC, 128, x.shape[1]).transpose(1, 0, 2))


def host_prep(inp, L):
    f32 = np.float32
    out = {}
    win_a = np.empty((L, 16, 128, NKC, 384), f32)
    win_v = np.empty((L, 2, 128, NKC, 512), f32)
    win_l = np.empty((L, 128, NKC, 320), f32)
    wout = np.empty((L, 4, 128, NKC, 512), f32)
    wg = np.empty((L, NG, 128, NKC, 512), f32)
    wu = np.empty((L, NG, 128, NKC, 512), f32)
    wd = np.empty((L, NG, 128, 4, D), f32)
    par = np.zeros((L, 128, NPAR), f32)
    lup = np.zeros((L, 3, 128, 1024), f32)
    gvec = np.zeros((2 * L + 1, D), f32)
    ar = np.arange(128)
    for l in range(L):
        w = inp["w_in"][l]
        if l > 0:
            w = np.concatenate([w, inp["w_in_vres"][l - 1]], axis=1)
        else:
            w = np.concatenate([w, np.zeros((D, 32), f32)], axis=1)
        for h in range(8):
            cols = np.concatenate([h * 128 + ar, 1024 + h * 128 + ar, 3072 + h * 128 + ar])
            win_a[l, h] = _blk(w, cols)
            cols = 4096 + np.concatenate([h * 128 + ar, 1024 + h * 128 + ar, 2048 + h * 128 + ar])
            win_a[l, 8 + h] = _blk(w, cols)
        for cb in range(2):
            win_v[l, cb] = _blk(w, 2048 + cb * 512 + np.arange(512))
        win_l[l] = _blk(w, 7168 + np.arange(320))
        for cb in range(4):
            wout[l, cb] = _blk(inp["w_out"][l], cb * 512 + np.arange(512))
        for g in range(NG):
            wg[l, g] = _blk(inp["w_gate"][l], g * 512 + np.arange(512))
            wu[l, g] = _blk(inp["w_up"][l], g * 512 + np.arange(512))
            wd[l, g] = inp["w_down"][l][g * 512:(g + 1) * 512].reshape(4, 128, D).transpose(1, 0, 2)
        mu = inp["mu_shift"][l]
        if l > 0:
            mu = np.concatenate([mu, inp["mu_shift_vres"][l - 1]])
        else:
            mu = np.concatenate([mu, np.zeros(32, f32)])
        for fb in range(8):
            par[l, :, fb] = mu[fb * 128:(fb + 1) * 128]
            par[l, :, 8 + fb] = mu[1024 + fb * 128:1024 + (fb + 1) * 128]
            par[l, :, 16 + fb] = mu[2048 + fb * 128:2048 + (fb + 1) * 128]
        par[l, :, 24] = mu[3072:3200]
        par[l, :, 25] = mu[3200:3328]
        par[l, :64, 26] = mu[3328:3392]

        def pk(v, c0):
            par[l, :, c0:c0 + 8] = v.reshape(8, 128).T

        pk(inp["rwkv_w0"][l], 27)
        pk(inp["rwkv_a0"][l], 35)
        if l > 0:
            pk(inp["rwkv_v0"][l - 1], 43)
        pk(inp["rwkv_k_k"][l], 51)
        pk(inp["rwkv_k_a"][l], 59)
        pk(inp["rwkv_r_k"][l].reshape(-1), 67)
        pk(inp["rwkv_ln_w"][l], 75)
        pk(inp["rwkv_ln_b"][l], 83)
        pk(inp["ret_ln_w"][l], 91)
        lup[l, 0, :64] = inp["rwkv_w_up"][l]
        lup[l, 0, 64:] = inp["rwkv_a_up"][l]
        lup[l, 1] = inp["rwkv_g_up"][l][:128]
        lup[l, 2, :32] = inp["rwkv_g_up"][l][128:160]
        if l > 0:
            lup[l, 2, 32:64] = inp["rwkv_v_up"][l - 1]
        gvec[2 * l] = inp["g_attn"][l]
        gvec[2 * l + 1] = inp["g_ffn"][l]
    gvec[2 * L] = inp["g_final"]
    out.update(win_a=win_a, win_v=win_v, win_l=win_l, wout=wout, wg=wg, wu=wu, wd=wd,
               par=par, lup=lup, gvec=gvec)
    return out


def host_consts(T):
    f32 = np.float32
    c = {}
    c["ident"] = np.eye(128, dtype=f32)
    ob = np.zeros((128, 128), f32)
    ob[:64, :64] = 1
    ob[64:, 64:] = 1
    c["onesblk"] = ob
    c["ones"] = np.ones((128, 128), f32)
    c["i64s"] = np.concatenate([np.eye(64, dtype=f32), np.eye(64, dtype=f32)], axis=0)
    jj = np.arange(128)
    c["maskT"] = (jj[:, None] <= jj[None, :]).astype(f32)
    rot = np.zeros((128, 128), f32)
    for dp in range(64):
        rot[dp + 64, dp] = -1.0
        rot[dp, dp + 64] = 1.0
    c["rotT"] = rot
    half = 64
    inv = (np.float32(10000.0) ** (-(np.arange(half, dtype=f32)) / np.float32(half))).astype(f32)
    def tabs(pos):
        ang = (pos[:, None].astype(f32) * inv[None, :]).astype(f32)
        cs = np.cos(ang).astype(f32).T
        sn = np.sin(ang).astype(f32).T
        return np.concatenate([cs, cs], 0), np.concatenate([sn, sn], 0)
    cs, sn = tabs(np.arange(T, dtype=f32))
    sc = np.float32(128.0 ** -0.5)
    c["rope_p"] = np.ascontiguousarray(np.stack([cs, sn, cs * sc, sn * sc], 1))
    cs, sn = tabs(np.full(SPC, 16384.0, dtype=f32))
    c["rope_s"] = np.ascontiguousarray(np.stack([cs, sn, cs * sc, sn * sc], 1))
    lg = np.log1p(-np.exp2(-5.0 - np.arange(8, dtype=np.float64)))
    j = np.arange(128, dtype=np.float64)
    g1 = np.exp(lg[None, :] * (-j[:, None] - 1.0))
    g2 = np.exp(lg[None, :] * (127.0 - j[:, None]))
    c["gtab"] = np.ascontiguousarray(np.stack([g1, g2], 1).astype(f32))
    eps = 1e-5 / np.exp(lg[:, None] * 2.0 * (j[None, :] + 1.0))
    c["epst"] = np.ascontiguousarray(np.broadcast_to(eps[None].astype(f32), (128, 8, 128)))
    c["eye16"] = np.eye(16, dtype=f32)
    return c


def build(L, T, with_sample=True):
    NT = T // TT
    nc = bass.Bass("TRN2", target_bir_lowering=False)
    dr = {}

    def din(name, shape):
        dr[name] = nc.dram_tensor(name, list(shape), F32, kind="ExternalInput").ap()
        return dr[name]

    def dout(name, shape):
        dr[name] = nc.dram_tensor(name, list(shape), F32, kind="ExternalOutput").ap()
        return dr[name]

    din("xp", (T, D)); din("xs", (SPCORE, D))
    din("sret", (L, SPCORE, 8, 128, 128)); din("srw", (L, SPCORE, 16, 64, 64)); din("ssh", (L, SPCORE, D))
    din("win_a", (L, 16, 128, NKC, 384)); din("win_v", (L, 2, 128, NKC, 512)); din("win_l", (L, 128, NKC, 320))
    din("wout", (L, 4, 128, NKC, 512)); din("wg", (L, NG, 128, NKC, 512)); din("wu", (L, NG, 128, NKC, 512))
    din("wd", (L, NG, 128, 4, D)); din("par", (L, 128, NPAR)); din("lup", (L, 3, 128, 1024))
    din("gvec", (2 * L + 1, D))
    for n, s in (("ident", (128, 128)), ("onesblk", (128, 128)), ("ones", (128, 128)), ("i64s", (128, 64)),
                 ("maskT", (128, 128)), ("rotT", (128, 128)), ("rope_p", (128, 4, T)), ("rope_s", (128, 4, SPC)),
                 ("gtab", (128, 2, 8)), ("epst", (128, 8, 128)), ("eye16", (16, 16))):
        din(n, s)
    dout("yp", (T, D)); dout("ys", (SPCORE, D))
    dout("retp", (L, 8, 128, 128)); dout("rwp", (L, 16, 64, 64)); dout("shp", (L, D))
    dout("rets", (L, SPCORE, 8, 128, 128)); dout("rws", (L, SPCORE, 16, 64, 64)); dout("shs", (L, SPCORE, D))

    with contextlib.ExitStack() as stack:
        S = Sched(nc, stack)
        sb = S.sb
        ident = sb("ident", (128, 128)); onesblk = sb("onesblk", (128, 128)); ones = sb("ones", (128, 128))
        i64s = sb("i64s", (128, 64)); maskT = sb("maskT", (128, 128)); rotT = sb("rotT", (128, 128))
        gtab = sb("gtab", (128, 2, 8)); epst = sb("epst", (128, 8, 128)); eye16 = sb("eye16", (16, 16))
        epsc = sb("epsc", (128, 128)); epsw = sb("epsw", (128, 128))
        rope = sb("rope", (128, 4, TT))
        for tl, n in ((ident, "ident"), (onesblk, "onesblk"), (ones, "ones"), (i64s, "i64s"), (maskT, "maskT"),
                      (rotT, "rotT"), (gtab, "gtab"), (epst, "epst"), (eye16, "eye16")):
            S.dma("sp", tl[:], dr[n], writes=[tl])
        S.op("dve", lambda e: e.memset(epsc[:], 1e-5), writes=[epsc])
        S.op("dve", lambda e: e.memset(epsw[:], 64e-5), writes=[epsw])
        par = sb("par", (128, NPAR)); omka = sb("omka", (128, 8))
        lup = [sb(f"lup{i}", (128, 1024)) for i in range(3)]
        gb = sb("gb", (128, D))
        NSLOT = 3
        slots = [sb(f"wslot{i}", (128, NKC * 512), BF16) for i in range(NSLOT)]
        slot_rr = [0]
        h = sb("h", (128, D)); xn = sb("xn", (128, D))
        xnT = sb("xnT", (128, NKC, TT), BF16); yT = sb("yT", (128, NKC, TT), BF16)
        ss = sb("ss", (128, 1)); rstd = sb("rstd", (128, 1))
        psb = [Tl(stack.enter_context(nc.psum_tensor(f"ps{i}", [128, 512], F32)), f"ps{i}") for i in range(8)]

        def ps():
            b = psb[S.ps_rr % 7]
            S.ps_rr += 1
            return b

        Sret = [[sb(f"Sret{l}_{hh}", (128, 128)) for hh in range(8)] for l in range(L)]
        Srw = [sb(f"Srw{l}", (128, 8, 64)) for l in range(L)]
        carry = [sb(f"carry{l}", (128, 32)) for l in range(L)]
        for l in range(L):
            for hh in range(8):
                S.op("dve", lambda e, t=Sret[l][hh]: e.memset(t[:], 0.0), writes=[Sret[l][hh]])
            S.op("dve", lambda e, t=Srw[l]: e.memset(t[:], 0.0), writes=[Srw[l]])
            S.op("dve", lambda e, t=carry[l]: e.memset(t[:], 0.0), writes=[carry[l]])
        RW = {n: sb("rw_" + n, (128, 8, TT)) for n in ("R", "KM", "V", "DEC", "KK", "B", "VF", "O")}
        LX = [sb(f"LX{i}", (128, TT)) for i in range(3)]
        P = [sb(f"P{i}", (128, TT + 1)) for i in range(2)]
        p_rr = [0]
        tmp = [sb(f"tmp{i}", (128, 512)) for i in range(6)]
        t_rr = [0]
        DG = [sb(f"DG{i}", (128, 8, 64)) for i in range(3)]
        dg_rr = [0]
        sa = sb("sa", (128, 8))
        kraw_t = sb("kraw", (128, TT)); At_t = sb("At", (128, TT)); yn_t = sb("yn", (128, TT))
        v1 = sb("v1", (128, 8, 128), BF16); v2 = sb("v2", (128, 8, 128), BF16); vs = sb("vs", (16, 1024))
        qf = sb("qf", (128, TT)); kf = sb("kf", (128, TT)); SG = sb("SG", (128, TT))
        qT = sb("qT", (128, TT), BF16); kT = sb("kT", (128, TT), BF16); ktm = sb("ktm", (128, 128), BF16)
        ktm32 = sb("ktm32", (16, 128)); kfr = sb("kfr", (128, TT)); qfr = sb("qfr", (128, TT))
        sTm = sb("sTm", (128, 128), BF16); yf = sb("yf", (128, TT))
        Vm = [sb(f"Vm{i}", (16, 128)) for i in range(2)]
        S0 = [sb(f"S0_{i}", (128, 128)) for i in range(3)]
        S1 = [sb(f"S1_{i}", (128, 128)) for i in range(3)]
        STs = [sb(f"STs{i}", (128, 8, 64)) for i in range(2)]
        actT = [sb(f"actT{i}", (128, 4, TT), BF16) for i in range(2)]
        sgt = sb("sgt", (128, TT))

        def T6():
            t = tmp[t_rr[0] % 6]
            t_rr[0] += 1
            return t

        def wload(src_ap, ncols_total, view):
            sl = slots[slot_rr[0] % NSLOT]
            slot_rr[0] += 1
            S.dma("pool", view(sl), src_ap, writes=[sl])
            return sl

        def act(fn, reads, writes):
            S.op("act", fn, reads, writes)

        def dve(fn, reads, writes):
            S.op("dve", fn, reads, writes)

        def mm(out, lhsT, rhs, start, stop, reads, writes, inc=None):
            S.op("pe", lambda e: e.matmul(out, lhsT, rhs, start=start, stop=stop), reads, writes,
                 inc=(stop if inc is None else inc))

        def norm(n, gidx, shift_out=None):
            S.dma("sp", gb[0:n, :], dr["gvec"][gidx:gidx + 1, :].to_broadcast([n, D]), writes=[gb])
            dve(lambda e: e.memset(ss[0:n, :], 0.0), [], [ss])
            act(lambda e: e.activation(out=xn[0:n, :], in_=h[0:n, :], func=AF.Square, accum_out=ss[0:n, :]),
                [h, ss], [xn, ss])
            dve(lambda e: e.tensor_scalar(out=rstd[0:n, :], in0=ss[0:n, :], scalar1=1.0 / D, scalar2=1e-6,
                                          op0=ALU.mult, op1=ALU.add), [ss], [rstd])
            act(lambda e: e.activation(out=rstd[0:n, :], in_=rstd[0:n, :], func=AF.Sqrt), [rstd], [rstd])
            dve(lambda e: e.reciprocal(out=rstd[0:n, :], in_=rstd[0:n, :]), [rstd], [rstd])
            dve(lambda e: e.scalar_tensor_tensor(out=xn[0:n, :], in0=h[0:n, :], scalar=rstd[0:n, 0:1],
                                                 in1=gb[0:n, :], op0=ALU.mult, op1=ALU.mult),
                [h, rstd, gb], [xn])
            if shift_out is not None:
                S.dma("sp", shift_out, xn[(n - 1 if n == 128 else 0):n, :], reads=[xn])

        def to_fm(src, n, dst, c0):
            for g in range(4):
                b = ps()
                for i in range(4):
                    kc = g * 4 + i
                    S.op("pe", lambda e, kc=kc, i=i, b=b: e.transpose(b[:, i * 128:i * 128 + n],
                                                                       src[0:n, kc * 128:(kc + 1) * 128],
                                                                       ident[0:n, 0:n]),
                         [src, ident], [b], inc=(i == 3))
                eng = "act" if g % 2 == 0 else "dve"
                o = dst[:, g * 4:(g + 1) * 4, c0:c0 + n]
                i_ = b[:, :].rearrange("p (a c) -> p a c", a=4)[:, :, 0:n]
                if eng == "act":
                    act(lambda e, o=o, i_=i_: e.activation(out=o, in_=i_, func=AF.Copy), [b], [dst])
                else:
                    dve(lambda e, o=o, i_=i_: e.tensor_copy(out=o, in_=i_), [b], [dst])

        def gemm_fm(sl, wview, c0, m, rhsT, n, nk=NKC):
            b = ps()
            for kc in range(nk):
                mm(b[0:m, 0:n], wview[:, kc, c0:c0 + m], rhsT[:, kc, 0:n], kc == 0, kc == nk - 1,
                   [sl, rhsT], [b])
            return b

        def tshift(b, m, n, mode, mucol, cl, cidx, out_ap, out_tl):
            Pb = P[p_rr[0] % 2]
            p_rr[0] += 1
            if mode == "p":
                act(lambda e: e.activation(out=Pb[0:m, 1:n + 1], in_=b[0:m, 0:n], func=AF.Copy), [b], [Pb])
                act(lambda e: e.activation(out=Pb[0:m, 0:1], in_=cl[0:m, cidx:cidx + 1], func=AF.Copy),
                    [cl, Pb], [Pb])
                act(lambda e: e.activation(out=cl[0:m, cidx:cidx + 1], in_=Pb[0:m, n:n + 1], func=AF.Copy),
                    [Pb, cl], [cl])
                t = T6()
                dve(lambda e: e.tensor_tensor(out=t[0:m, 0:n], in0=Pb[0:m, 0:n], in1=Pb[0:m, 1:n + 1],
                                              op=ALU.subtract), [Pb], [t])
                dve(lambda e: e.scalar_tensor_tensor(out=out_ap, in0=t[0:m, 0:n], scalar=par[0:m, mucol:mucol + 1],
                                                     in1=Pb[0:m, 1:n + 1], op0=ALU.mult, op1=ALU.add),
                    [t, par, Pb], [out_tl])
            else:
                act(lambda e: e.activation(out=Pb[0:m, 0:2 * n], in_=b[0:m, 0:2 * n], func=AF.Copy), [b], [Pb])
                t = T6()
                dve(lambda e: e.tensor_tensor(out=t[0:m, 0:n], in0=Pb[0:m, n:2 * n], in1=Pb[0:m, 0:n],
                                              op=ALU.subtract), [Pb], [t])
                dve(lambda e: e.scalar_tensor_tensor(out=out_ap, in0=t[0:m, 0:n], scalar=par[0:m, mucol:mucol + 1],
                                                     in1=Pb[0:m, 0:n], op0=ALU.mult, op1=ALU.add),
                    [t, par, Pb], [out_tl])

        def headnorm_fm(src_tl, src_ap, n, red, inv_cnt, eps_ap, eps_tl, out_tl, out_ap):
            b1 = ps()
            mm(b1[:, 0:n], red[:], src_ap, True, True, [red, src_tl], [b1])
            sq = T6()
            act(lambda e: e.activation(out=sq[:, 0:n], in_=src_ap, func=AF.Square), [src_tl], [sq])
            b2 = ps()
            mm(b2[:, 0:n], red[:], sq[:, 0:n], True, True, [red, sq], [b2])
            mean = T6()
            dve(lambda e: e.tensor_scalar(out=mean[:, 0:n], in0=b1[:, 0:n], scalar1=inv_cnt, scalar2=None,
                                          op0=ALU.mult), [b1], [mean])
            msq = T6()
            dve(lambda e: e.tensor_tensor(out=msq[:, 0:n], in0=mean[:, 0:n], in1=mean[:, 0:n], op=ALU.mult),
                [mean], [msq])
            var = T6()
            dve(lambda e: e.scalar_tensor_tensor(out=var[:, 0:n], in0=b2[:, 0:n], scalar=inv_cnt, in1=msq[:, 0:n],
                                                 op0=ALU.mult, op1=ALU.subtract), [b2, msq], [var])
            dve(lambda e: e.tensor_tensor(out=var[:, 0:n], in0=var[:, 0:n], in1=eps_ap, op=ALU.add),
                [var, eps_tl], [var])
            act(lambda e: e.activation(out=var[:, 0:n], in_=var[:, 0:n], func=AF.Sqrt), [var], [var])
            dve(lambda e: e.reciprocal(out=var[:, 0:n], in_=var[:, 0:n]), [var], [var])
            dve(lambda e: e.tensor_tensor(out=out_ap, in0=src_ap, in1=mean[:, 0:n], op=ALU.subtract),
                [src_tl, mean], [out_tl])
            dve(lambda e: e.tensor_tensor(out=out_ap, in0=out_ap, in1=var[:, 0:n], op=ALU.mult),
                [out_tl, var], [out_tl])

        def layer(l, mode, n, tile_idx, last_tile):
            nrhs = n if mode == "p" else 2 * n
            S.dma("sp", par[:], dr["par"][l], writes=[par])
            for i in range(3):
                S.dma("sp", lup[i][:], dr["lup"][l, i], writes=[lup[i]])
            dve(lambda e: e.tensor_scalar(out=omka[:], in0=par[:, 59:67], scalar1=-1.0, scalar2=1.0,
                                          op0=ALU.mult, op1=ALU.add), [par], [omka])
            sh_out = None
            if mode == "p" and last_tile:
                sh_out = dr["shp"][l:l + 1, :]
            if mode == "s":
                sh_out = dr["shs"][l, tile_idx * SPC:(tile_idx + 1) * SPC, :]
            norm(n, 2 * l, sh_out)
            to_fm(xn, n, xnT, 0)
            if mode == "s":
                S.dma("sp", xn[0:n, :], dr["ssh"][l, tile_idx * SPC:(tile_idx + 1) * SPC, :], reads=[], writes=[xn])
                to_fm(xn, n, xnT, n)
            for cb in range(2):
                sl = wload(dr["win_v"][l, cb], 512, lambda s: s[:, :].rearrange("p (k c) -> p k c", k=NKC))
                wv = sl[:, :].rearrange("p (k c) -> p k c", k=NKC)
                b = ps()
                for kc in range(NKC):
                    mm(b[0:n, :], xnT[:, kc, 0:n], wv[:, kc, :], kc == 0, kc == NKC - 1, [sl, xnT], [b])
                if mode == "p":
                    for vi, vt in ((0, v1), (1, v2)):
                        dve(lambda e, vi=vi, vt=vt, b=b, cb=cb: e.tensor_tensor(
                            out=vt[:, cb * 4:(cb + 1) * 4, :], in0=b[:, :].rearrange("p (a c) -> p a c", a=4),
                            in1=gtab[:, vi, cb * 4:(cb + 1) * 4].unsqueeze(2).to_broadcast([128, 4, 128]),
                            op=ALU.mult), [b, gtab], [vt])
                else:
                    act(lambda e, b=b, cb=cb: e.activation(out=vs[0:n, cb * 512:(cb + 1) * 512], in_=b[0:n, :],
                                                           func=AF.Copy), [b], [vs])
            for hh in range(8):
                sl = wload(dr["win_a"][l, hh], 384, lambda s: s[:, 0:NKC * 384].rearrange("p (k c) -> p k c", k=NKC))
                wv = sl[:, 0:NKC * 384].rearrange("p (k c) -> p k c", k=NKC)
                bq = gemm_fm(sl, wv, 0, 128, xnT, n)
                bk = gemm_fm(sl, wv, 128, 128, xnT, n)
                bg = gemm_fm(sl, wv, 256, 128, xnT, n)
                act(lambda e, bq=bq: e.activation(out=qf[:, 0:n], in_=bq[:, 0:n], func=AF.Copy), [bq], [qf])
                act(lambda e, bk=bk: e.activation(out=kf[:, 0:n], in_=bk[:, 0:n], func=AF.Copy), [bk], [kf])
                act(lambda e, bg=bg: e.activation(out=SG[:, 0:n], in_=bg[:, 0:n], func=AF.Silu), [bg], [SG])
                for src, dstf, dstb, ci in ((qf, qfr, qT, 0), (kf, kfr, kT, 2)):
                    br = ps()
                    mm(br[:, 0:n], rotT[:], src[:, 0:n], True, True, [rotT, src], [br])
                    t = T6()
                    dve(lambda e, t=t, src=src, ci=ci: e.tensor_tensor(out=t[:, 0:n], in0=src[:, 0:n],
                                                                        in1=rope[:, ci, 0:n], op=ALU.mult),
                        [src, rope], [t])
                    t2 = T6()
                    dve(lambda e, t2=t2, br=br, ci=ci: e.tensor_tensor(out=t2[:, 0:n], in0=br[:, 0:n],
                                                                        in1=rope[:, ci + 1, 0:n], op=ALU.mult),
                        [br, rope], [t2])
                    dve(lambda e, t=t, t2=t2, dstf=dstf: e.tensor_tensor(out=dstf[:, 0:n], in0=t[:, 0:n],
                                                                          in1=t2[:, 0:n], op=ALU.add),
                        [t, t2], [dstf])
                    act(lambda e, dstf=dstf, dstb=dstb: e.activation(out=dstb[:, 0:n], in_=dstf[:, 0:n],
                                                                     func=AF.Copy), [dstf], [dstb])
                by = psb[7]
                if mode == "p":
                    bt = ps()
                    S.op("pe", lambda e, bt=bt: e.transpose(bt[:, 0:128], kfr[:, 0:128], ident[:, :]),
                         [kfr, ident], [bt])
                    act(lambda e, bt=bt: e.activation(out=ktm[:, :], in_=bt[:, 0:128], func=AF.Copy), [bt], [ktm])
                    bs = ps()
                    mm(bs[:, 0:128], kT[:, 0:128], qT[:, 0:128], True, True, [kT, qT], [bs])
                    dve(lambda e, bs=bs: e.tensor_tensor(out=sTm[:, :], in0=bs[:, 0:128], in1=maskT[:, :],
                                                         op=ALU.mult), [bs, maskT], [sTm])
                    mm(by[:, 0:128], v1[:, hh, :], sTm[:, :], True, False, [v1, sTm], [by])
                    mm(by[:, 0:128], Sret[l][hh][:, :], qfr[:, 0:128], False, True, [Sret[l][hh], qfr], [by])
                    bkv = ps()
                    mm(bkv[:, 0:128], ktm[:, :], v2[:, hh, :], True, True, [ktm, v2], [bkv])
                    St = Sret[l][hh]
                    dve(lambda e, St=St, bkv=bkv, hh=hh: e.scalar_tensor_tensor(
                        out=St[:, :], in0=St[:, :], scalar=float(GAM[hh] ** 128), in1=bkv[:, 0:128],
                        op0=ALU.mult, op1=ALU.add), [St, bkv], [St])
                    if last_tile:
                        S.dma("sp", dr["retp"][l, hh], St[:, :], reads=[St])
                    eps_ap, eps_tl = epst[:, hh, 0:n], epst
                else:
                    bt = ps()
                    S.op("pe", lambda e, bt=bt: e.transpose(bt[0:n, 0:128], kfr[:, 0:n], ident[:, :]),
                         [kfr, ident], [bt])
                    act(lambda e, bt=bt: e.activation(out=ktm32[0:n, :], in_=bt[0:n, 0:128], func=AF.Copy),
                        [bt], [ktm32])
                    for t in range(n):
                        s0 = S0[t % 3]
                        s1 = S1[t % 3]
                        S.dma("sp", s0[:, :], dr["sret"][l, tile_idx * SPC + t, hh], writes=[s0])
                        vm = Vm[t % 2]
                        dve(lambda e, vm=vm, hh=hh, t=t: e.tensor_scalar(
                            out=vm[:, :], in0=vs[0:16, hh * 128:(hh + 1) * 128], scalar1=eye16[:, t:t + 1],
                            scalar2=None, op0=ALU.mult), [vs, eye16], [vm])
                        bkv = ps()
                        mm(bkv[:, 0:128], ktm32[0:n, :], vm[:, :], True, True, [ktm32, vm], [bkv])
                        dve(lambda e, s0=s0, s1=s1, bkv=bkv, hh=hh: e.scalar_tensor_tensor(
                            out=s1[:, :], in0=s0[:, :], scalar=float(GAM[hh]), in1=bkv[:, 0:128],
                            op0=ALU.mult, op1=ALU.add), [s0, bkv], [s1])
                        S.dma("sp", dr["rets"][l, tile_idx * SPC + t, hh], s1[:, :], reads=[s1])
                        mm(by[:, t:t + 1], s1[:, :], qfr[:, t:t + 1], True, True, [s1, qfr], [by], inc=(t == n - 1))
                    eps_ap, eps_tl = epsc[:, 0:n], epsc
                act(lambda e, by=by: e.activation(out=yf[:, 0:n], in_=by[:, 0:n], func=AF.Copy), [by], [yf])
                yn = yn_t
                headnorm_fm(yf, yf[:, 0:n], n, ones, 1.0 / 128, eps_ap, eps_tl, yn, yn[:, 0:n])
                dve(lambda e, yn=yn, hh=hh: e.scalar_tensor_tensor(
                    out=yT[:, hh, 0:n], in0=yn[:, 0:n], scalar=par[:, 91 + hh:92 + hh], in1=SG[:, 0:n],
                    op0=ALU.mult, op1=ALU.mult), [yn, par, SG], [yT])
            sl = wload(dr["win_l"][l], 320, lambda s: s[:, 0:NKC * 320].rearrange("p (k c) -> p k c", k=NKC))
            wv = sl[:, 0:NKC * 320].rearrange("p (k c) -> p k c", k=NKC)
            for sbk, m in ((0, 128), (1, 128), (2, 64)):
                b = gemm_fm(sl, wv, sbk * 128, m, xnT, nrhs)
                tshift(b, m, n, mode, 24 + sbk, carry[l], 24 + sbk, LX[sbk][0:m, 0:n], LX[sbk])
            act(lambda e: e.activation(out=LX[0][0:64, 0:n], in_=LX[0][0:64, 0:n], func=AF.Tanh), [LX[0]], [LX[0]])
            act(lambda e: e.activation(out=LX[1][:, 0:n], in_=LX[1][:, 0:n], func=AF.Sigmoid), [LX[1]], [LX[1]])
            act(lambda e: e.activation(out=LX[2][0:32, 0:n], in_=LX[2][0:32, 0:n], func=AF.Sigmoid), [LX[2]], [LX[2]])
            R, KM, V, DEC, KK, B, VF, O = (RW[k] for k in ("R", "KM", "V", "DEC", "KK", "B", "VF", "O"))
            for fb in range(8):
                fc = slice(fb * 128, (fb + 1) * 128)
                sl = wload(dr["win_a"][l, 8 + fb], 384, lambda s: s[:, 0:NKC * 384].rearrange("p (k c) -> p k c", k=NKC))
                wv = sl[:, 0:NKC * 384].rearrange("p (k c) -> p k c", k=NKC)
                for j, arr in ((0, R), (1, None), (2, V)):
                    b = gemm_fm(sl, wv, j * 128, 128, xnT, nrhs)
                    if arr is None:
                        kraw = kraw_t
                        tshift(b, 128, n, mode, 8 * j + fb, carry[l], 8 * j + fb, kraw[:, 0:n], kraw)
                    else:
                        tshift(b, 128, n, mode, 8 * j + fb, carry[l], 8 * j + fb, arr[:, fb, 0:n], arr)
                b = ps()
                mm(b[:, 0:n], lup[0][0:64, fc], LX[0][0:64, 0:n], True, True, [lup[0], LX[0]], [b])
                t = T6()
                act(lambda e, b=b, t=t, fb=fb: e.activation(out=t[:, 0:n], in_=b[:, 0:n], func=AF.Sigmoid,
                                                            bias=par[:, 27 + fb:28 + fb]), [b, par], [t])
                act(lambda e, t=t, fb=fb: e.activation(out=DEC[:, fb, 0:n], in_=t[:, 0:n], func=AF.Exp,
                                                       scale=-C_DEC), [t], [DEC])
                b = ps()
                mm(b[:, 0:n], lup[0][64:128, fc], LX[0][64:128, 0:n], True, True, [lup[0], LX[0]], [b])
                At = At_t
                act(lambda e, b=b, fb=fb, At=At: e.activation(out=At[:, 0:n], in_=b[:, 0:n], func=AF.Sigmoid,
                                                              bias=par[:, 35 + fb:36 + fb]), [b, par], [At])
                if l == 0:
                    act(lambda e, fb=fb: e.activation(out=VF[:, fb, 0:n], in_=V[:, fb, 0:n], func=AF.Copy), [V], [VF])
                else:
                    b = ps()
                    mm(b[:, 0:n], lup[2][32:64, fc], LX[2][32:64, 0:n], True, True, [lup[2], LX[2]], [b])
                    vg = T6()
                    act(lambda e, b=b, vg=vg, fb=fb: e.activation(out=vg[:, 0:n], in_=b[:, 0:n], func=AF.Sigmoid,
                                                                  bias=par[:, 43 + fb:44 + fb]), [b, par], [vg])
                    dd = T6()
                    dve(lambda e, dd=dd, fb=fb: e.tensor_tensor(out=dd[:, 0:n], in0=VF[:, fb, 0:n], in1=V[:, fb, 0:n],
                                                                op=ALU.subtract), [VF, V], [dd])
                    dve(lambda e, dd=dd, vg=vg: e.tensor_tensor(out=dd[:, 0:n], in0=dd[:, 0:n], in1=vg[:, 0:n],
                                                                op=ALU.mult), [dd, vg], [dd])
                    dve(lambda e, dd=dd, fb=fb: e.tensor_tensor(out=V[:, fb, 0:n], in0=V[:, fb, 0:n], in1=dd[:, 0:n],
                                                                op=ALU.add), [V, dd], [V])
                kx = T6()
                dve(lambda e, kx=kx, kraw=kraw, fb=fb: e.tensor_scalar(out=kx[:, 0:n], in0=kraw[:, 0:n],
                                                                       scalar1=par[:, 51 + fb:52 + fb], scalar2=None,
                                                                       op0=ALU.mult), [kraw, par], [kx])
                sq = T6()
                act(lambda e, sq=sq, kx=kx: e.activation(out=sq[:, 0:n], in_=kx[:, 0:n], func=AF.Square), [kx], [sq])
                b = ps()
                mm(b[:, 0:n], onesblk[:, :], sq[:, 0:n], True, True, [onesblk, sq], [b])
                rn = T6()
                dve(lambda e, rn=rn, b=b: e.tensor_scalar(out=rn[:, 0:n], in0=b[:, 0:n], scalar1=1e-24, scalar2=None,
                                                          op0=ALU.add), [b], [rn])
                act(lambda e, rn=rn: e.activation(out=rn[:, 0:n], in_=rn[:, 0:n], func=AF.Sqrt), [rn], [rn])
                dve(lambda e, rn=rn: e.reciprocal(out=rn[:, 0:n], in_=rn[:, 0:n]), [rn], [rn])
                dve(lambda e, kx=kx, rn=rn, fb=fb: e.tensor_tensor(out=KK[:, fb, 0:n], in0=kx[:, 0:n], in1=rn[:, 0:n],
                                                                   op=ALU.mult), [kx, rn], [KK])
                t = T6()
                dve(lambda e, t=t, fb=fb, At=At: e.tensor_scalar(out=t[:, 0:n], in0=At[:, 0:n],
                                                                 scalar1=par[:, 59 + fb:60 + fb], scalar2=omka[:, fb:fb + 1],
                                                                 op0=ALU.mult, op1=ALU.add), [At, par, omka], [t])
                dve(lambda e, t=t, kraw=kraw, fb=fb: e.tensor_tensor(out=KM[:, fb, 0:n], in0=kraw[:, 0:n],
                                                                     in1=t[:, 0:n], op=ALU.mult), [kraw, t], [KM])
                dve(lambda e, fb=fb, At=At: e.tensor_tensor(out=B[:, fb, 0:n], in0=KK[:, fb, 0:n], in1=At[:, 0:n],
                                                            op=ALU.mult), [KK, At], [B])
            for t in range(n):
                if mode == "p":
                    ST = Srw[l]
                else:
                    ST = STs[t % 2]
                    S.dma("sp", ST[:, :, :], dr["srw"][l, tile_idx * SPC + t].rearrange("(f h) v k -> (h v) f k", h=2), writes=[ST])
                bc = []
                for i, arr in enumerate((KK, DEC, B, KM, R)):
                    dg = DG[dg_rr[0] % 3]
                    dg_rr[0] += 1
                    S.op("pool", lambda e, dg=dg, arr=arr, t=t: e.tensor_tensor(
                        out=dg[:, :, :], in0=arr[:, :, t:t + 1].to_broadcast([128, 8, 64]),
                        in1=i64s[:, :].unsqueeze(1).to_broadcast([128, 8, 64]), op=ALU.mult),
                        [arr, i64s], [dg])
                    b = ps()
                    mm(b[:, :], onesblk[:, :], dg[:, :, :].rearrange("p a c -> p (a c)"), True, True,
                       [onesblk, dg], [b])
                    bc.append(b)
                bKK, bDEC, bB, bKM, bR = bc
                ta = T6(); tb = T6(); tc = T6()

                def v3(x):
                    return x[:, :].rearrange("p (a c) -> p a c", a=8)

                dve(lambda e, ta=ta, ST=ST, b=bKK: e.tensor_tensor(out=v3(ta), in0=ST[:, :, :], in1=v3(b), op=ALU.mult),
                    [ST, bKK], [ta])
                dve(lambda e, ta=ta: e.tensor_reduce(out=sa[:, :], in_=v3(ta), axis=AX.X, op=ALU.add), [ta], [sa])
                dve(lambda e, tb=tb, ST=ST, b=bDEC: e.tensor_tensor(out=v3(tb), in0=ST[:, :, :], in1=v3(b), op=ALU.mult),
                    [ST, bDEC], [tb])
                dve(lambda e, tc=tc, b=bB: e.tensor_tensor(out=v3(tc), in0=v3(b),
                                                           in1=sa[:, :].unsqueeze(2).to_broadcast([128, 8, 64]),
                                                           op=ALU.mult), [bB, sa], [tc])
                dve(lambda e, tb=tb, tc=tc: e.tensor_tensor(out=tb[:, :], in0=tb[:, :], in1=tc[:, :], op=ALU.subtract),
                    [tb, tc], [tb])
                dve(lambda e, tc=tc, b=bKM, t=t: e.tensor_tensor(out=v3(tc), in0=v3(b),
                                                                 in1=V[:, :, t:t + 1].to_broadcast([128, 8, 64]),
                                                                 op=ALU.mult), [bKM, V], [tc])
                dve(lambda e, ST=ST, tb=tb, tc=tc: e.tensor_tensor(out=ST[:, :, :], in0=v3(tb), in1=v3(tc), op=ALU.add),
                    [tb, tc], [ST])
                dve(lambda e, ta=ta, ST=ST, b=bR: e.tensor_tensor(out=v3(ta), in0=ST[:, :, :], in1=v3(b), op=ALU.mult),
                    [ST, bR], [ta])
                dve(lambda e, ta=ta, t=t: e.tensor_reduce(out=O[:, :, t:t + 1], in_=v3(ta), axis=AX.X, op=ALU.add),
                    [ta], [O])
                if mode == "s":
                    S.dma("sp", dr["rws"][l, tile_idx * SPC + t].rearrange("(f h) v k -> (h v) f k", h=2), ST[:, :, :], reads=[ST])
            if mode == "p" and last_tile:
                S.dma("sp", dr["rwp"][l].rearrange("(f h) v k -> (h v) f k", h=2), Srw[l][:, :, :], reads=[Srw[l]])
            for fb in range(8):
                yn = yn_t
                headnorm_fm(O, O[:, fb, 0:n], n, onesblk, 1.0 / 64, epsw[:, 0:n], epsw, yn, yn[:, 0:n])
                dve(lambda e, yn=yn, fb=fb: e.tensor_scalar(out=yn[:, 0:n], in0=yn[:, 0:n],
                                                            scalar1=par[:, 75 + fb:76 + fb],
                                                            scalar2=par[:, 83 + fb:84 + fb], op0=ALU.mult, op1=ALU.add),
                    [yn, par], [yn])
                fc = slice(fb * 128, (fb + 1) * 128)
                t = T6()
                dve(lambda e, t=t, fb=fb: e.scalar_tensor_tensor(out=t[:, 0:n], in0=R[:, fb, 0:n],
                                                                 scalar=par[:, 67 + fb:68 + fb], in1=KM[:, fb, 0:n],
                                                                 op0=ALU.mult, op1=ALU.mult), [R, par, KM], [t])
                b = ps()
                mm(b[:, 0:n], onesblk[:, :], t[:, 0:n], True, True, [onesblk, t], [b])
                bon = T6()
                dve(lambda e, b=b, fb=fb, bon=bon: e.tensor_tensor(out=bon[:, 0:n], in0=b[:, 0:n], in1=V[:, fb, 0:n],
                                                                   op=ALU.mult), [b, V], [bon])
                dve(lambda e, yn=yn, bon=bon: e.tensor_tensor(out=yn[:, 0:n], in0=yn[:, 0:n], in1=bon[:, 0:n],
                                                              op=ALU.add), [yn, bon], [yn])
                bg = ps()
                mm(bg[:, 0:n], lup[1][:, fc], LX[1][:, 0:n], True, False, [lup[1], LX[1]], [bg])
                mm(bg[:, 0:n], lup[2][0:32, fc], LX[2][0:32, 0:n], False, True, [lup[2], LX[2]], [bg])
                dve(lambda e, yn=yn, fb=fb, bg=bg: e.tensor_tensor(out=yT[:, 8 + fb, 0:n], in0=yn[:, 0:n], in1=bg[:, 0:n],
                                                                   op=ALU.mult), [yn, bg], [yT])
            for cb in range(4):
                sl = wload(dr["wout"][l, cb], 512, lambda s: s[:, :].rearrange("p (k c) -> p k c", k=NKC))
                wv = sl[:, :].rearrange("p (k c) -> p k c", k=NKC)
                b = ps()
                for kc in range(NKC):
                    mm(b[0:n, :], yT[:, kc, 0:n], wv[:, kc, :], kc == 0, kc == NKC - 1, [sl, yT], [b])
                dve(lambda e, b=b, cb=cb: e.tensor_tensor(out=h[0:n, cb * 512:(cb + 1) * 512],
                                                          in0=h[0:n, cb * 512:(cb + 1) * 512], in1=b[0:n, :],
                                                          op=ALU.add), [h, b], [h])
            norm(n, 2 * l + 1)
            to_fm(xn, n, xnT, 0)
            for g in range(NG):
                slg = wload(dr["wg"][l, g], 512, lambda s: s[:, :].rearrange("p (k c) -> p k c", k=NKC))
                slu = wload(dr["wu"][l, g], 512, lambda s: s[:, :].rearrange("p (k c) -> p k c", k=NKC))
                sld = wload(dr["wd"][l, g], 512, lambda s: s[:, :].rearrange("p (k c) -> p k c", k=4))
                wgv = slg[:, :].rearrange("p (k c) -> p k c", k=NKC)
                wuv = slu[:, :].rearrange("p (k c) -> p k c", k=NKC)
                wdv = sld[:, :].rearrange("p (k c) -> p k c", k=4)
                aT = actT[g % 2]
                for j in range(4):
                    b1 = gemm_fm(slg, wgv, j * 128, 128, xnT, n)
                    b2 = gemm_fm(slu, wuv, j * 128, 128, xnT, n)
                    act(lambda e, b1=b1: e.activation(out=sgt[:, 0:n], in_=b1[:, 0:n], func=AF.Silu), [b1], [sgt])
                    dve(lambda e, b2=b2, j=j, aT=aT: e.tensor_tensor(out=aT[:, j, 0:n], in0=sgt[:, 0:n],
                                                                      in1=b2[:, 0:n], op=ALU.mult), [sgt, b2], [aT])
                for cb in range(4):
                    b = ps()
                    for kc in range(4):
                        mm(b[0:n, :], aT[:, kc, 0:n], wdv[:, kc, cb * 512:(cb + 1) * 512], kc == 0, kc == 3,
                           [sld, aT], [b])
                    dve(lambda e, b=b, cb=cb: e.tensor_tensor(out=h[0:n, cb * 512:(cb + 1) * 512],
                                                              in0=h[0:n, cb * 512:(cb + 1) * 512], in1=b[0:n, :],
                                                              op=ALU.add), [h, b], [h])

        for ti in range(NT):
            S.dma("sp", h[:, :], dr["xp"][ti * TT:(ti + 1) * TT, :], writes=[h])
            S.dma("sp", rope[:, :, :], dr["rope_p"][:, :, ti * TT:(ti + 1) * TT], writes=[rope])
            for l in range(L):
                layer(l, "p", TT, ti, ti == NT - 1)
            norm(TT, 2 * L)
            S.dma("sp", dr["yp"][ti * TT:(ti + 1) * TT, :], xn[:, :], reads=[xn])
        if with_sample:
            for sg in range(NSG):
                S.dma("sp", h[0:SPC, :], dr["xs"][sg * SPC:(sg + 1) * SPC, :], writes=[h])
                S.dma("sp", rope[:, :, 0:SPC], dr["rope_s"], writes=[rope])
                for l in range(L):
                    layer(l, "s", SPC, sg, False)
                norm(SPC, 2 * L)
                S.dma("sp", dr["ys"][sg * SPC:(sg + 1) * SPC, :], xn[0:SPC, :], reads=[xn])
        S.finish()

        with nc.Block() as block:
            @block.tensor
            def _(e):
                for f in S.E["pe"].q:
                    f(e)

            @block.scalar
            def _(e):
                for f in S.E["act"].q:
                    f(e)

            @block.vector
            def _(e):
                for f in S.E["dve"].q:
                    f(e)

            @block.gpsimd
            def _(e):
                for f in S.E["pool"].q:
                    f(e)

            @block.sync
            def _(e):
                for f in S.E["sp"].q:
                    f(e)
    return nc


def run(inp, L, T, B, with_sample=True):
    import time as _t
    _t0 = _t.time()
    hp = host_prep(inp, L)
    cs = host_consts(T)
    print("host_prep s", _t.time() - _t0, flush=True)
    _t0 = _t.time()
    nc = build(L, T, with_sample)
    print("build s", _t.time() - _t0, flush=True)
    _t0 = _t.time()
    in_maps = []
    for c in range(NCORE):
        m = dict(hp)
        m.update(cs)
        b = c % B
        m["xp"] = np.ascontiguousarray(inp["x_prompt"][b, :T])
        m["xs"] = np.ascontiguousarray(inp["x_sample"][c * SPCORE:(c + 1) * SPCORE, 0])
        m["sret"] = np.ascontiguousarray(inp["state_ret"][:L, c * SPCORE:(c + 1) * SPCORE])
        m["srw"] = np.ascontiguousarray(inp["state_rwkv"][:L, c * SPCORE:(c + 1) * SPCORE])
        m["ssh"] = np.ascontiguousarray(inp["state_shift"][:L, c * SPCORE:(c + 1) * SPCORE])
        in_maps.append(m)
    res = run_bass_kernel_spmd(nc, in_maps, core_ids=list(range(NCORE)))
    print("spmd s", _t.time() - _t0, flush=True)
    r = res.results
    yp = np.stack([r[b]["yp"] for b in range(B)])
    ys = np.concatenate([r[c]["ys"] for c in range(NCORE)])[:, None, :]
    retp = np.stack([r[b]["retp"] for b in range(B)], 1)
    rwp = np.stack([r[b]["rwp"] for b in range(B)], 1)
    shp = np.stack([r[b]["shp"] for b in range(B)], 1)
    rets = np.concatenate([r[c]["rets"] for c in range(NCORE)], 1)
    rws = np.concatenate([r[c]["rws"] for c in range(NCORE)], 1)
    shs = np.concatenate([r[c]["shs"] for c in range(NCORE)], 1)
    return (yp, ys, retp, rwp, shp, rets, rws, shs)


def kernel(**inputs):
    inp = {k: np.asarray(v) for k, v in inputs.items()}
    L = inp["w_in"].shape[0]
    B, T = inp["x_prompt"].shape[0], inp["x_prompt"].shape[1]
    return run(inp, L, T, B)
```

```python
import contextlib
import numpy as np
import concourse.bass as bass
import concourse.mybir as mybir
from concourse.bass_utils import run_bass_kernel_spmd

F32 = mybir.dt.float32
BF16 = mybir.dt.bfloat16
AF = mybir.ActivationFunctionType
ALU = mybir.AluOpType
AX = mybir.AxisListType

D = 2048
NKC = 16
DFF = 5632
NG = 11
RETW = 1024
SPC = 16
NSG = 2
NCORE = 4
SPCORE = SPC * NSG
TT = 128
NPAR = 104
C_DEC = float(np.exp(-0.5))
GAM = [1.0 - 2.0 ** (-5.0 - h) for h in range(8)]


class Sem:
    def __init__(self, h):
        self.h = h
        self.val = 0


class Eng:
    def __init__(self, name, sem):
        self.name = name
        self.sem = sem
        self.q = []
        self.seen = {}


class Tl:
    def __init__(self, t, name):
        self.t = t
        self.name = name
        self.w = None
        self.r = {}

    def __getitem__(self, k):
        return self.t[k]


class Sched:
    def __init__(self, nc, stack, ndma_sp=20, ndma_pool=8):
        self.nc = nc
        self.stack = stack
        self.E = {}
        for n in ("pe", "act", "dve", "pool", "sp"):
            self.E[n] = Eng(n, Sem(stack.enter_context(nc.semaphore("s_" + n))))
        self.dsem = {
            "sp": [Sem(stack.enter_context(nc.semaphore(f"dsp{i}"))) for i in range(ndma_sp)],
            "pool": [Sem(stack.enter_context(nc.semaphore(f"dpl{i}"))) for i in range(ndma_pool)],
        }
        self.dcnt = {"sp": 0, "pool": 0}
        self.nalloc = 0
        self.ps_rr = 0

    def sb(self, name, shape, dt=F32):
        t = self.stack.enter_context(self.nc.sbuf_tensor("sb_" + name, list(shape), dt))
        return Tl(t, name)

    def _wait(self, E, ev):
        sem, val = ev
        if E.seen.get(id(sem), 0) >= val:
            return
        E.seen[id(sem)] = val
        E.q.append(lambda e, h=sem.h, v=val: e.wait_ge(h, v))

    def _deps(self, E, reads, writes, selfsync):
        evs = []
        for b in reads:
            if b.w is not None:
                evs.append(b.w)
        for b in writes:
            if b.w is not None:
                evs.append(b.w)
            evs.extend(b.r.values())
        for ev in evs:
            if (not selfsync) and ev[0] is E.sem:
                continue
            self._wait(E, ev)

    def _reg(self, me, reads, writes):
        for b in reads:
            b.r[id(me[0])] = me
        for b in writes:
            b.w = me
            b.r = {}

    def op(self, en, fn, reads=(), writes=(), inc=True):
        E = self.E[en]
        self._deps(E, reads, writes, selfsync=(en != "pe"))
        if inc:
            E.sem.val += 1
            me = (E.sem, E.sem.val)
            E.q.append(lambda e, f=fn, h=E.sem.h: f(e).then_inc(h, 1))
        else:
            me = (E.sem, E.sem.val + 1)
            E.q.append(lambda e, f=fn: f(e))
        self._reg(me, reads, writes)

    def dma(self, qn, out, in_, reads=(), writes=()):
        E = self.E[qn]
        pool = self.dsem[qn]
        sem = pool[self.dcnt[qn] % len(pool)]
        self.dcnt[qn] += 1
        self._deps(E, reads, writes, selfsync=True)
        if sem.val > 0:
            self._wait(E, (sem, sem.val))
        sem.val += 16
        me = (sem, sem.val)
        E.q.append(lambda e, o=out, i=in_, h=sem.h: e.dma_start(out=o, in_=i).then_inc(h, 16))
        self._reg(me, reads, writes)

    def finish(self):
        E = self.E["sp"]
        for qn in ("sp", "pool"):
            for sem in self.dsem[qn]:
                if sem.val > 0:
                    self._wait(E, (sem, sem.val))
        for n in ("pe", "act", "dve", "pool"):
            s = self.E[n].sem
            if s.val > 0:
                self._wait(E, (s, s.val))


def _blk(w, cols):
    x = w[:, cols]
    return np.ascontiguousarray(x.reshape(NKC, 128, x.shape[1]).transpose(1, 0, 2))


def host_prep(inp, L):
    f32 = np.float32
    out = {}
    win_a = np.empty((L, 16, 128, NKC, 384), f32)
    win_v = np.empty((L, 2, 128, NKC, 512), f32)
    win_l = np.empty((L, 128, NKC, 320), f32)
    wout = np.empty((L, 4, 128, NKC, 512), f32)
    wg = np.empty((L, NG, 128, NKC, 512), f32)
    wu = np.empty((L, NG, 128, NKC, 512), f32)
    wd = np.empty((L, NG, 128, 4, D), f32)
    par = np.zeros((L, 128, NPAR), f32)
    lup = np.zeros((L, 3, 128, 1024), f32)
    gvec = np.zeros((2 * L + 1, D), f32)
    ar = np.arange(128)
    for l in range(L):
        w = inp["w_in"][l]
        if l > 0:
            w = np.concatenate([w, inp["w_in_vres"][l - 1]], axis=1)
        else:
            w = np.concatenate([w, np.zeros((D, 32), f32)], axis=1)
        for h in range(8):
            cols = np.concatenate([h * 128 + ar, 1024 + h * 128 + ar, 3072 + h * 128 + ar])
            win_a[l, h] = _blk(w, cols)
            cols = 4096 + np.concatenate([h * 128 + ar, 1024 + h * 128 + ar, 2048 + h * 128 + ar])
            win_a[l, 8 + h] = _blk(w, cols)
        for cb in range(2):
            win_v[l, cb] = _blk(w, 2048 + cb * 512 + np.arange(512))
        win_l[l] = _blk(w, 7168 + np.arange(320))
        for cb in range(4):
            wout[l, cb] = _blk(inp["w_out"][l], cb * 512 + np.arange(512))
        for g in range(NG):
            wg[l, g] = _blk(inp["w_gate"][l], g * 512 + np.arange(512))
            wu[l, g] = _blk(inp["w_up"][l], g * 512 + np.arange(512))
            wd[l, g] = inp["w_down"][l][g * 512:(g + 1) * 512].reshape(4, 128, D).transpose(1, 0, 2)
        mu = inp["mu_shift"][l]
        if l > 0:
            mu = np.concatenate([mu, inp["mu_shift_vres"][l - 1]])
        else:
            mu = np.concatenate([mu, np.zeros(32, f32)])
        for fb in range(8):
            par[l, :, fb] = mu[fb * 128:(fb + 1) * 128]
            par[l, :, 8 + fb] = mu[1024 + fb * 128:1024 + (fb + 1) * 128]
            par[l, :, 16 + fb] = mu[2048 + fb * 128:2048 + (fb + 1) * 128]
        par[l, :, 24] = mu[3072:3200]
        par[l, :, 25] = mu[3200:3328]
        par[l, :64, 26] = mu[3328:3392]

        def pk(v, c0):
            par[l, :, c0:c0 + 8] = v.reshape(8, 128).T

        pk(inp["rwkv_w0"][l], 27)
        pk(inp["rwkv_a0"][l], 35)
        if l > 0:
            pk(inp["rwkv_v0"][l - 1], 43)
        pk(inp["rwkv_k_k"][l], 51)
        pk(inp["rwkv_k_a"][l], 59)
        pk(inp["rwkv_r_k"][l].reshape(-1), 67)
        pk(inp["rwkv_ln_w"][l], 75)
        pk(inp["rwkv_ln_b"][l], 83)
        pk(inp["ret_ln_w"][l], 91)
        lup[l, 0, :64] = inp["rwkv_w_up"][l]
        lup[l, 0, 64:] = inp["rwkv_a_up"][l]
        lup[l, 1] = inp["rwkv_g_up"][l][:128]
        lup[l, 2, :32] = inp["rwkv_g_up"][l][128:160]
        if l > 0:
            lup[l, 2, 32:64] = inp["rwkv_v_up"][l - 1]
        gvec[2 * l] = inp["g_attn"][l]
        gvec[2 * l + 1] = inp["g_ffn"][l]
    gvec[2 * L] = inp["g_final"]
    out.update(win_a=win_a, win_v=win_v, win_l=win_l, wout=wout, wg=wg, wu=wu, wd=wd,
               par=par, lup=lup, gvec=gvec)
    return out


def host_consts(T):
    f32 = np.float32
    c = {}
    c["ident"] = np.eye(128, dtype=f32)
    ob = np.zeros((128, 128), f32)
    ob[:64, :64] = 1
    ob[64:, 64:] = 1
    c["onesblk"] = ob
    c["ones"] = np.ones((128, 128), f32)
    c["i64s"] = np.concatenate([np.eye(64, dtype=f32), np.eye(64, dtype=f32)], axis=0)
    jj = np.arange(128)
    c["maskT"] = (jj[:, None] <= jj[None, :]).astype(f32)
    rot = np.zeros((128, 128), f32)
    for dp in range(64):
        rot[dp + 64, dp] = -1.0
        rot[dp, dp + 64] = 1.0
    c["rotT"] = rot
    half = 64
    inv = (np.float32(10000.0) ** (-(np.arange(half, dtype=f32)) / np.float32(half))).astype(f32)
    def tabs(pos):
        ang = (pos[:, None].astype(f32) * inv[None, :]).astype(f32)
        cs = np.cos(ang).astype(f32).T
        sn = np.sin(ang).astype(f32).T
        return np.concatenate([cs, cs], 0), np.concatenate([sn, sn], 0)
    cs, sn = tabs(np.arange(T, dtype=f32))
    sc = np.float32(128.0 ** -0.5)
    c["rope_p"] = np.ascontiguousarray(np.stack([cs, sn, cs * sc, sn * sc], 1))
    cs, sn = tabs(np.full(SPC, 16384.0, dtype=f32))
    c["rope_s"] = np.ascontiguousarray(np.stack([cs, sn, cs * sc, sn * sc], 1))
    lg = np.log1p(-np.exp2(-5.0 - np.arange(8, dtype=np.float64)))
    j = np.arange(128, dtype=np.float64)
    g1 = np.exp(lg[None, :] * (-j[:, None] - 1.0))
    g2 = np.exp(lg[None, :] * (127.0 - j[:, None]))
    c["gtab"] = np.ascontiguousarray(np.stack([g1, g2], 1).astype(f32))
    eps = 1e-5 / np.exp(lg[:, None] * 2.0 * (j[None, :] + 1.0))
    c["epst"] = np.ascontiguousarray(np.broadcast_to(eps[None].astype(f32), (128, 8, 128)))
    c["eye16"] = np.eye(16, dtype=f32)
    a64 = np.arange(128) % 64
    blk = (np.arange(128)[:, None] // 64) == (np.arange(128)[None, :] // 64)
    mST = (a64[:, None] < a64[None, :]).astype(f32)
    mIT = (a64[:, None] <= a64[None, :]).astype(f32)
    mSL = (a64[:, None] > a64[None, :]).astype(f32)
    UT = (blk & (a64[:, None] <= a64[None, :])).astype(f32)
    c["cm"] = np.ascontiguousarray(np.stack([-mST, -mSL, mST, mIT, UT], 1))
    return c


def build(L, T, with_sample=True):
    NT = T // TT
    nc = bass.Bass("TRN2", target_bir_lowering=False)
    dr = {}

    def din(name, shape):
        dr[name] = nc.dram_tensor(name, list(shape), F32, kind="ExternalInput").ap()
        return dr[name]

    def dout(name, shape):
        dr[name] = nc.dram_tensor(name, list(shape), F32, kind="ExternalOutput").ap()
        return dr[name]

    din("xp", (T, D)); din("xs", (SPCORE, D))
    din("sret", (L, SPCORE, 8, 128, 128)); din("srw", (L, SPCORE, 16, 64, 64)); din("ssh", (L, SPCORE, D))
    din("win_a", (L, 16, 128, NKC, 384)); din("win_v", (L, 2, 128, NKC, 512)); din("win_l", (L, 128, NKC, 320))
    din("wout", (L, 4, 128, NKC, 512)); din("wg", (L, NG, 128, NKC, 512)); din("wu", (L, NG, 128, NKC, 512))
    din("wd", (L, NG, 128, 4, D)); din("par", (L, 128, NPAR)); din("lup", (L, 3, 128, 1024))
    din("gvec", (2 * L + 1, D))
    for n, s in (("ident", (128, 128)), ("onesblk", (128, 128)), ("ones", (128, 128)), ("i64s", (128, 64)),
                 ("maskT", (128, 128)), ("rotT", (128, 128)), ("rope_p", (128, 4, T)), ("rope_s", (128, 4, SPC)),
                 ("gtab", (128, 2, 8)), ("epst", (128, 8, 128)), ("eye16", (16, 16)), ("cm", (128, 5, 128))):
        din(n, s)
    dout("yp", (T, D)); dout("ys", (SPCORE, D))
    dout("retp", (L, 8, 128, 128)); dout("rwp", (L, 16, 64, 64)); dout("shp", (L, D))
    dout("rets", (L, SPCORE, 8, 128, 128)); dout("rws", (L, SPCORE, 16, 64, 64)); dout("shs", (L, SPCORE, D))

    with contextlib.ExitStack() as stack:
        S = Sched(nc, stack)
        sb = S.sb
        ident = sb("ident", (128, 128)); onesblk = sb("onesblk", (128, 128)); ones = sb("ones", (128, 128))
        i64s = sb("i64s", (128, 64)); maskT = sb("maskT", (128, 128)); rotT = sb("rotT", (128, 128))
        gtab = sb("gtab", (128, 2, 8)); ept = [sb(f"ept{i}", (128, 128)) for i in range(2)]; eye16 = sb("eye16", (16, 16))
        cm = sb("cm", (128, 5, 128))
        epsc = sb("epsc", (128, 128)); epsw = sb("epsw", (128, 128))
        rope = sb("rope", (128, 4, TT))
        for tl, n in ((ident, "ident"), (onesblk, "onesblk"), (ones, "ones"), (i64s, "i64s"), (maskT, "maskT"),
                      (rotT, "rotT"), (gtab, "gtab"), (cm, "cm"), (eye16, "eye16")):
            S.dma("sp", tl[:], dr[n], writes=[tl])
        S.op("dve", lambda e: e.memset(epsc[:], 1e-5), writes=[epsc])
        S.op("dve", lambda e: e.memset(epsw[:], 64e-5), writes=[epsw])
        par = sb("par", (128, NPAR)); omka = sb("omka", (128, 8))
        lupb = [[sb(f"lup{i}_{j}", (128, 128)) for i in range(3)] for j in range(2)]
        lup_rr = [0]
        gb = sb("gb", (128, D))
        NSLOT = 3
        slots = [sb(f"wslot{i}", (128, NKC * 512), BF16) for i in range(NSLOT)]
        slot_rr = [0]
        h = sb("h", (128, D)); xn = sb("xn", (128, D))
        xnT = sb("xnT", (128, NKC, TT), BF16); yT = sb("yT", (128, NKC, TT), BF16)
        ss = sb("ss", (128, 1)); rstd = sb("rstd", (128, 1))
        psb = [Tl(stack.enter_context(nc.psum_tensor(f"ps{i}", [128, 512], F32)), f"ps{i}") for i in range(8)]

        def ps():
            b = psb[S.ps_rr % 7]
            S.ps_rr += 1
            return b

        Sret = [[sb(f"Sret{l}_{hh}", (128, 128)) for hh in range(8)] for l in range(L)]
        Tst = [sb(f"Tst{l}", (128, 8, 64)) for l in range(L)]
        Tbd = sb("Tbd", (128, 128))
        carry = [sb(f"carry{l}", (128, 32)) for l in range(L)]
        for l in range(L):
            for hh in range(8):
                S.op("dve", lambda e, t=Sret[l][hh]: e.memset(t[:], 0.0), writes=[Sret[l][hh]])
            S.op("dve", lambda e, t=Tst[l]: e.memset(t[:], 0.0), writes=[Tst[l]])
            S.op("dve", lambda e, t=carry[l]: e.memset(t[:], 0.0), writes=[carry[l]])
        RW = {n: sb("rw_" + n, (128, 8, TT)) for n in ("R", "KM", "V", "DEC", "KK", "B", "VF", "O")}
        LX = [sb(f"LX{i}", (128, TT)) for i in range(3)]
        P = [sb(f"P{i}", (128, TT + 1)) for i in range(2)]
        p_rr = [0]
        tmp = [sb(f"tmp{i}", (128, 512)) for i in range(6)]
        t_rr = [0]
        DG = [sb(f"DG{i}", (128, 8, 64)) for i in range(3)]
        dg_rr = [0]
        sa = sb("sa", (128, 8))
        kraw_t = sb("kraw", (128, TT)); At_t = sb("At", (128, TT)); yn_t = sb("yn", (128, TT))
        v1 = sb("v1", (128, 8, 128), BF16); v2 = sb("v2", (128, 8, 128), BF16); vs = sb("vs", (16, 1024))
        qf = sb("qf", (128, TT)); kf = sb("kf", (128, TT)); SG = sb("SG", (128, TT))
        qT = sb("qT", (128, TT), BF16); kT = sb("kT", (128, TT), BF16); ktm = sb("ktm", (128, 128), BF16)
        ktm32 = sb("ktm32", (16, 128)); kfr = sb("kfr", (128, TT)); qfr = sb("qfr", (128, TT))
        sTm = sb("sTm", (128, 128), BF16); yf = sb("yf", (128, TT))
        Vm = [sb(f"Vm{i}", (16, 128)) for i in range(2)]
        S0 = [sb(f"S0_{i}", (128, 128)) for i in range(3)]
        S1 = [sb(f"S1_{i}", (128, 128)) for i in range(3)]
        STs = [sb(f"STs{i}", (128, 8, 64)) for i in range(2)]
        actT = [sb(f"actT{i}", (128, 4, TT), BF16) for i in range(2)]
        sgt = sb("sgt", (128, TT))

        def T6():
            t = tmp[t_rr[0] % 6]
            t_rr[0] += 1
            return t

        def wload(src_ap, ncols_total, view):
            sl = slots[slot_rr[0] % NSLOT]
            slot_rr[0] += 1
            S.dma("pool", view(sl), src_ap, writes=[sl])
            return sl

        def act(fn, reads, writes):
            S.op("act", fn, reads, writes)

        def dve(fn, reads, writes):
            S.op("dve", fn, reads, writes)

        def mm(out, lhsT, rhs, start, stop, reads, writes, inc=None):
            S.op("pe", lambda e: e.matmul(out, lhsT, rhs, start=start, stop=stop), reads, writes,
                 inc=(stop if inc is None else inc))

        def norm(n, gidx, shift_out=None):
            S.dma("sp", gb[0:n, :], dr["gvec"][gidx:gidx + 1, :].to_broadcast([n, D]), writes=[gb])
            dve(lambda e: e.memset(ss[0:n, :], 0.0), [], [ss])
            act(lambda e: e.activation(out=xn[0:n, :], in_=h[0:n, :], func=AF.Square, accum_out=ss[0:n, :]),
                [h, ss], [xn, ss])
            dve(lambda e: e.tensor_scalar(out=rstd[0:n, :], in0=ss[0:n, :], scalar1=1.0 / D, scalar2=1e-6,
                                          op0=ALU.mult, op1=ALU.add), [ss], [rstd])
            act(lambda e: e.activation(out=rstd[0:n, :], in_=rstd[0:n, :], func=AF.Sqrt), [rstd], [rstd])
            dve(lambda e: e.reciprocal(out=rstd[0:n, :], in_=rstd[0:n, :]), [rstd], [rstd])
            dve(lambda e: e.scalar_tensor_tensor(out=xn[0:n, :], in0=h[0:n, :], scalar=rstd[0:n, 0:1],
                                                 in1=gb[0:n, :], op0=ALU.mult, op1=ALU.mult),
                [h, rstd, gb], [xn])
            if shift_out is not None:
                S.dma("sp", shift_out, xn[(n - 1 if n == 128 else 0):n, :], reads=[xn])

        def to_fm(src, n, dst, c0):
            for g in range(4):
                b = ps()
                for i in range(4):
                    kc = g * 4 + i
                    S.op("pe", lambda e, kc=kc, i=i, b=b: e.transpose(b[:, i * 128:i * 128 + n],
                                                                       src[0:n, kc * 128:(kc + 1) * 128],
                                                                       ident[0:n, 0:n]),
                         [src, ident], [b], inc=(i == 3))
                eng = "act" if g % 2 == 0 else "dve"
                o = dst[:, g * 4:(g + 1) * 4, c0:c0 + n]
                i_ = b[:, :].rearrange("p (a c) -> p a c", a=4)[:, :, 0:n]
                if eng == "act":
                    act(lambda e, o=o, i_=i_: e.activation(out=o, in_=i_, func=AF.Copy), [b], [dst])
                else:
                    dve(lambda e, o=o, i_=i_: e.tensor_copy(out=o, in_=i_), [b], [dst])

        def gemm_fm(sl, wview, c0, m, rhsT, n, nk=NKC):
            b = ps()
            for kc in range(nk):
                mm(b[0:m, 0:n], wview[:, kc, c0:c0 + m], rhsT[:, kc, 0:n], kc == 0, kc == nk - 1,
                   [sl, rhsT], [b])
            return b

        def tshift(b, m, n, mode, mucol, cl, cidx, out_ap, out_tl):
            Pb = P[p_rr[0] % 2]
            p_rr[0] += 1
            if mode == "p":
                act(lambda e: e.activation(out=Pb[0:m, 1:n + 1], in_=b[0:m, 0:n], func=AF.Copy), [b], [Pb])
                act(lambda e: e.activation(out=Pb[0:m, 0:1], in_=cl[0:m, cidx:cidx + 1], func=AF.Copy),
                    [cl, Pb], [Pb])
                act(lambda e: e.activation(out=cl[0:m, cidx:cidx + 1], in_=Pb[0:m, n:n + 1], func=AF.Copy),
                    [Pb, cl], [cl])
                t = T6()
                dve(lambda e: e.tensor_tensor(out=t[0:m, 0:n], in0=Pb[0:m, 0:n], in1=Pb[0:m, 1:n + 1],
                                              op=ALU.subtract), [Pb], [t])
                dve(lambda e: e.scalar_tensor_tensor(out=out_ap, in0=t[0:m, 0:n], scalar=par[0:m, mucol:mucol + 1],
                                                     in1=Pb[0:m, 1:n + 1], op0=ALU.mult, op1=ALU.add),
                    [t, par, Pb], [out_tl])
            else:
                act(lambda e: e.activation(out=Pb[0:m, 0:2 * n], in_=b[0:m, 0:2 * n], func=AF.Copy), [b], [Pb])
                t = T6()
                dve(lambda e: e.tensor_tensor(out=t[0:m, 0:n], in0=Pb[0:m, n:2 * n], in1=Pb[0:m, 0:n],
                                              op=ALU.subtract), [Pb], [t])
                dve(lambda e: e.scalar_tensor_tensor(out=out_ap, in0=t[0:m, 0:n], scalar=par[0:m, mucol:mucol + 1],
                                                     in1=Pb[0:m, 0:n], op0=ALU.mult, op1=ALU.add),
                    [t, par, Pb], [out_tl])

        def headnorm_fm(src_tl, src_ap, n, red, inv_cnt, eps_ap, eps_tl, out_tl, out_ap):
            b1 = ps()
            mm(b1[:, 0:n], red[:], src_ap, True, True, [red, src_tl], [b1])
            sq = T6()
            act(lambda e: e.activation(out=sq[:, 0:n], in_=src_ap, func=AF.Square), [src_tl], [sq])
            b2 = ps()
            mm(b2[:, 0:n], red[:], sq[:, 0:n], True, True, [red, sq], [b2])
            mean = T6()
            dve(lambda e: e.tensor_scalar(out=mean[:, 0:n], in0=b1[:, 0:n], scalar1=inv_cnt, scalar2=None,
                                          op0=ALU.mult), [b1], [mean])
            msq = T6()
            dve(lambda e: e.tensor_tensor(out=msq[:, 0:n], in0=mean[:, 0:n], in1=mean[:, 0:n], op=ALU.mult),
                [mean], [msq])
            var = T6()
            dve(lambda e: e.scalar_tensor_tensor(out=var[:, 0:n], in0=b2[:, 0:n], scalar=inv_cnt, in1=msq[:, 0:n],
                                                 op0=ALU.mult, op1=ALU.subtract), [b2, msq], [var])
            dve(lambda e: e.tensor_tensor(out=var[:, 0:n], in0=var[:, 0:n], in1=eps_ap, op=ALU.add),
                [var, eps_tl], [var])
            act(lambda e: e.activation(out=var[:, 0:n], in_=var[:, 0:n], func=AF.Sqrt), [var], [var])
            dve(lambda e: e.reciprocal(out=var[:, 0:n], in_=var[:, 0:n]), [var], [var])
            dve(lambda e: e.tensor_tensor(out=out_ap, in0=src_ap, in1=mean[:, 0:n], op=ALU.subtract),
                [src_tl, mean], [out_tl])
            dve(lambda e: e.tensor_tensor(out=out_ap, in0=out_ap, in1=var[:, 0:n], op=ALU.mult),
                [out_tl, var], [out_tl])

        def flat8(tl):
            return tl[:, :, :].rearrange("p a c -> p (a c)")

        DECt = RW["DEC"]
        for tl_ in (DECt, DG[0], DG[1]):
            S.op("dve", lambda e, t=tl_: e.memset(t[:], 0.0), writes=[tl_])
        S.op("dve", lambda e: e.memset(Tbd[:], 0.0), writes=[Tbd])
        Abd = (DECt, DECt[:, 0:2, :]); Rbd = (DECt, DECt[:, 2:4, :])
        Bbd = (DECt, DECt[:, 4:6, :]); Kbd = (DECt, DECt[:, 6:8, :])
        g0 = flat8(DG[0]).rearrange("p (f c t) -> p f c t", f=2, c=2)
        g1 = flat8(DG[1]).rearrange("p (f c t) -> p f c t", f=2, c=2)
        g2 = flat8(DG[2]).rearrange("p (f t) -> p f t", f=4)
        VTbd = (DG[0], g0[:, 0]); Bpbd = (DG[0], g0[:, 1]); Kpbd = (DG[1], g1[:, 0])
        Uc = [(DG[1], g1[:, 1, c]) for c in range(2)]
        st_v = [flat8(STs[c]).rearrange("p (f t) -> p f t", f=4) for c in range(2)]
        Vbd = [(STs[c], st_v[c][:, 0]) for c in range(2)]
        Bptm = [(STs[c], st_v[c][:, 1]) for c in range(2)]
        Kptm = [(STs[c], st_v[c][:, 2]) for c in range(2)]
        AKT = [(STs[c], st_v[c][:, 3]) for c in range(2)]
        ARBT = [(DG[2], g2[:, 2 * c]) for c in range(2)]
        ARKT = [(DG[2], g2[:, 2 * c + 1]) for c in range(2)]
        xt = [sb(f"cx{i}", (128, 128)) for i in range(6)]
        NTa = [(S0[c], S0[c][:, :]) for c in range(2)]
        NTb = [(S1[c], S1[c][:, :]) for c in range(2)]
        Na = [(xt[c], xt[c][:, :]) for c in range(2)]
        Nb = [(xt[2 + c], xt[2 + c][:, :]) for c in range(2)]
        Lm = [(xt[4 + c], xt[4 + c][:, :]) for c in range(2)]
        AmT = [(S0[2], S0[2][:, :]), (S1[2], S1[2][:, :])]
        Zt = [[sb(f"Z{c}_{i}", (128, 256)) for i in range(2)] for c in range(2)]
        ev_rr = [0]

        def evac(dst, src_tl, src_ap):
            ev_rr[0] += 1
            if ev_rr[0] % 2 == 0:
                act(lambda e: e.activation(out=dst[1], in_=src_ap, func=AF.Copy), [src_tl], [dst[0]])
            else:
                dve(lambda e: e.tensor_copy(out=dst[1], in_=src_ap), [src_tl], [dst[0]])

        def tr(src, dst):
            b = ps()
            S.op("pe", lambda e: e.transpose(b[:, 0:128], src[1], ident[:, :]), [src[0], ident], [b])
            evac(dst, b, b[:, 0:128])

        def amat(lhs, rhs, mi, dst):
            b = ps()
            mm(b[:, 0:128], lhs[1], rhs[1], True, True, [lhs[0], rhs[0]], [b])
            dve(lambda e: e.tensor_tensor(out=dst[1], in0=b[:, 0:128], in1=cm[:, mi, :], op=ALU.mult),
                [b, cm], [dst[0]])

        def chunk_pair(l, fb):
            Ea, Eb, Ec = qf, kf, kfr
            R, KM, V, KK, B = (RW[k] for k in ("R", "KM", "V", "KK", "B"))
            c2 = lambda ap: ap.rearrange("p (c t) -> p c t", c=2)
            bt = ps()
            S.op("pe", lambda e: e.transpose(bt[:, 0:128], SG[:, 0:128], ident[:, :]), [SG, ident], [bt])
            act(lambda e: e.activation(out=yf[:, 0:128], in_=bt[:, 0:128], func=AF.Copy), [bt], [yf])
            bc_ = ps()
            mm(bc_[:, 0:128], yf[:, 0:128], cm[:, 4, :], True, True, [yf, cm], [bc_])
            act(lambda e: e.activation(out=Ea[:, 0:128], in_=bc_[:, 0:128], func=AF.Copy), [bc_], [Ea])

            def bdw(dst, src_tl, src_ap_fn, E_tl):
                for h2 in range(2):
                    r_ = slice(h2 * 64, (h2 + 1) * 64)
                    if E_tl is None:
                        S.op("pool", lambda e, r_=r_: e.tensor_copy(out=dst[1][r_, :, r_], in_=c2(src_ap_fn(r_))),
                             [src_tl], [dst[0]])
                    else:
                        S.op("pool", lambda e, r_=r_: e.tensor_tensor(out=dst[1][r_, :, r_], in0=c2(src_ap_fn(r_)),
                                                                      in1=c2(E_tl[r_, 0:128]), op=ALU.mult),
                             [src_tl, E_tl], [dst[0]])

            dve(lambda e: e.tensor_tensor(out=Eb[:, 0:128], in0=Ea[:, 0:128], in1=SG[:, 0:128], op=ALU.subtract),
                [Ea, SG], [Eb])
            act(lambda e: e.activation(out=Eb[:, 0:128], in_=Eb[:, 0:128], func=AF.Exp, scale=-C_DEC), [Eb], [Eb])
            bdw(Abd, KK, lambda r_: KK[r_, fb, 0:128], Eb)
            act(lambda e: e.activation(out=Ec[:, 0:128], in_=Ea[:, 0:128], func=AF.Exp, scale=-C_DEC), [Ea], [Ec])
            bdw(Rbd, R, lambda r_: R[r_, fb, 0:128], Ec)
            act(lambda e: e.activation(out=Eb[:, 0:128], in_=Ea[:, 0:128], func=AF.Exp, scale=C_DEC), [Ea], [Eb])
            bdw(Bbd, B, lambda r_: B[r_, fb, 0:128], Eb)
            bdw(Kbd, KM, lambda r_: KM[r_, fb, 0:128], Eb)
            dve(lambda e: e.tensor_tensor(out=c2(Eb[:, 0:128]), in0=c2(Ea[:, 0:128])[:, :, 63:64].to_broadcast([128, 2, 64]),
                                          in1=c2(Ea[:, 0:128]), op=ALU.subtract), [Ea], [Eb])
            act(lambda e: e.activation(out=Eb[:, 0:128], in_=Eb[:, 0:128], func=AF.Exp, scale=-C_DEC), [Eb], [Eb])
            bdw(Bpbd, B, lambda r_: B[r_, fb, 0:128], Eb)
            bdw(Kpbd, KM, lambda r_: KM[r_, fb, 0:128], Eb)
            bdw(VTbd, V, lambda r_: V[r_, fb, 0:128], None)
            Zc = [0, 0]
            for c in range(2):
                fam = lambda f: (f[0], f[1][:, c, :])
                tr(fam(VTbd), Vbd[c])
                tr(fam(Abd), (Zt[c][0], Zt[c][0][:, 0:128]))
                tr(fam(Bpbd), Bptm[c])
                tr(fam(Kpbd), Kptm[c])
                amat(fam(Bbd), fam(Abd), 0, NTa[c])
                amat(fam(Abd), fam(Bbd), 1, Na[c])
                amat(fam(Kbd), fam(Abd), 2, AKT[c])
                amat(fam(Bbd), fam(Rbd), 3, ARBT[c])
                amat(fam(Kbd), fam(Rbd), 3, ARKT[c])
            for c in range(2):
                b = ps()
                mm(b[:, 0:128], AKT[c][1], Vbd[c][1], True, True, [AKT[c][0], Vbd[c][0]], [b])
                evac((Zt[c][0], Zt[c][0][:, 128:256]), b, b[:, 0:128])
            NT = [NTa, NTb]
            NN = [Na, Nb]
            for i in range(6):
                cur, nxt = i % 2, (i + 1) % 2
                for c in range(2):
                    nt, nn = NT[cur][c], NN[cur][c]
                    dve(lambda e, nt=nt, c=c: e.tensor_tensor(out=Lm[c][1], in0=nt[1], in1=ident[:, :], op=ALU.add),
                        [nt[0], ident], [Lm[c][0]])
                    zc, zn = Zt[c][i % 2], Zt[c][(i + 1) % 2]
                    b = ps()
                    mm(b[:, 0:256], Lm[c][1], zc[:, 0:256], True, True, [Lm[c][0], zc], [b])
                    evac((zn, zn[:, 0:256]), b, b[:, 0:256])
                    if i < 5:
                        b2 = ps()
                        mm(b2[:, 0:128], nn[1], nt[1], True, True, [nn[0], nt[0]], [b2])
                        evac(NT[nxt][c], b2, b2[:, 0:128])
                    if i < 4:
                        b3 = ps()
                        mm(b3[:, 0:128], nt[1], nn[1], True, True, [nt[0], nn[0]], [b3])
                        evac(NN[nxt][c], b3, b3[:, 0:128])
            Zf = [Zt[c][0] for c in range(2)]
            for c in range(2):
                tr((Zf[c], Zf[c][:, 0:128]), AmT[c])
            for h2 in range(2):
                r_ = slice(h2 * 64, (h2 + 1) * 64)
                S.op("pool", lambda e, r_=r_: e.tensor_copy(out=Tbd[r_, r_], in_=Tst[l][r_, fb, :]), [Tst[l]], [Tbd])
            for c in range(2):
                b = ps()
                mm(b[:, 0:128], AmT[c][1], Tbd[:, :], True, True, [AmT[c][0], Tbd], [b])
                dve(lambda e, b=b, c=c: e.scalar_tensor_tensor(out=Uc[c][1], in0=b[:, 0:128], scalar=-1.0,
                                                               in1=Zf[c][:, 128:256], op0=ALU.mult, op1=ALU.subtract),
                    [b, Zf[c]], [Uc[c][0]])
                bo = ps()
                mm(bo[:, 0:128], Tbd[:, :], Rbd[1][:, c, :], True, False, [Tbd, Rbd[0]], [bo])
                mm(bo[:, 0:128], Uc[c][1], ARBT[c][1], False, False, [Uc[c][0], ARBT[c][0]], [bo])
                mm(bo[:, 0:128], Vbd[c][1], ARKT[c][1], False, True, [Vbd[c][0], ARKT[c][0]], [bo])
                for h2 in range(2):
                    r_ = slice(h2 * 64, (h2 + 1) * 64)
                    evac((RW["O"], RW["O"][r_, fb, c * 64:(c + 1) * 64]), bo, bo[r_, r_])
                bs_ = ps()
                mm(bs_[:, 0:128], Bptm[c][1], Uc[c][1], True, False, [Bptm[c][0], Uc[c][0]], [bs_])
                mm(bs_[:, 0:128], Kptm[c][1], Vbd[c][1], False, True, [Kptm[c][0], Vbd[c][0]], [bs_])
                dve(lambda e, bs_=bs_, c=c: e.scalar_tensor_tensor(out=Tbd[:, :], in0=Tbd[:, :],
                                                                   scalar=Ec[:, c * 64 + 63:c * 64 + 64],
                                                                   in1=bs_[:, 0:128], op0=ALU.mult, op1=ALU.add),
                    [Tbd, Ec, bs_], [Tbd])
            for h2 in range(2):
                r_ = slice(h2 * 64, (h2 + 1) * 64)
                S.op("pool", lambda e, r_=r_: e.tensor_copy(out=Tst[l][r_, fb, :], in_=Tbd[r_, r_]), [Tbd], [Tst[l]])

        def layer(l, mode, n, tile_idx, last_tile):
            nrhs = n if mode == "p" else 2 * n
            S.dma("sp", par[:], dr["par"][l], writes=[par])
            dve(lambda e: e.tensor_scalar(out=omka[:], in0=par[:, 59:67], scalar1=-1.0, scalar2=1.0,
                                          op0=ALU.mult, op1=ALU.add), [par], [omka])
            sh_out = None
            if mode == "p" and last_tile:
                sh_out = dr["shp"][l:l + 1, :]
            if mode == "s":
                sh_out = dr["shs"][l, tile_idx * SPC:(tile_idx + 1) * SPC, :]
            norm(n, 2 * l, sh_out)
            to_fm(xn, n, xnT, 0)
            if mode == "s":
                S.dma("sp", xn[0:n, :], dr["ssh"][l, tile_idx * SPC:(tile_idx + 1) * SPC, :], reads=[], writes=[xn])
                to_fm(xn, n, xnT, n)
            for cb in range(2):
                sl = wload(dr["win_v"][l, cb], 512, lambda s: s[:, :].rearrange("p (k c) -> p k c", k=NKC))
                wv = sl[:, :].rearrange("p (k c) -> p k c", k=NKC)
                b = ps()
                for kc in range(NKC):
                    mm(b[0:n, :], xnT[:, kc, 0:n], wv[:, kc, :], kc == 0, kc == NKC - 1, [sl, xnT], [b])
                if mode == "p":
                    for vi, vt in ((0, v1), (1, v2)):
                        dve(lambda e, vi=vi, vt=vt, b=b, cb=cb: e.tensor_tensor(
                            out=vt[:, cb * 4:(cb + 1) * 4, :], in0=b[:, :].rearrange("p (a c) -> p a c", a=4),
                            in1=gtab[:, vi, cb * 4:(cb + 1) * 4].unsqueeze(2).to_broadcast([128, 4, 128]),
                            op=ALU.mult), [b, gtab], [vt])
                else:
                    act(lambda e, b=b, cb=cb: e.activation(out=vs[0:n, cb * 512:(cb + 1) * 512], in_=b[0:n, :],
                                                           func=AF.Copy), [b], [vs])
            for hh in range(8):
                sl = wload(dr["win_a"][l, hh], 384, lambda s: s[:, 0:NKC * 384].rearrange("p (k c) -> p k c", k=NKC))
                wv = sl[:, 0:NKC * 384].rearrange("p (k c) -> p k c", k=NKC)
                bq = gemm_fm(sl, wv, 0, 128, xnT, n)
                bk = gemm_fm(sl, wv, 128, 128, xnT, n)
                bg = gemm_fm(sl, wv, 256, 128, xnT, n)
                act(lambda e, bq=bq: e.activation(out=qf[:, 0:n], in_=bq[:, 0:n], func=AF.Copy), [bq], [qf])
                act(lambda e, bk=bk: e.activation(out=kf[:, 0:n], in_=bk[:, 0:n], func=AF.Copy), [bk], [kf])
                act(lambda e, bg=bg: e.activation(out=SG[:, 0:n], in_=bg[:, 0:n], func=AF.Silu), [bg], [SG])
                for src, dstf, dstb, ci in ((qf, qfr, qT, 0), (kf, kfr, kT, 2)):
                    br = ps()
                    mm(br[:, 0:n], rotT[:], src[:, 0:n], True, True, [rotT, src], [br])
                    t = T6()
                    dve(lambda e, t=t, src=src, ci=ci: e.tensor_tensor(out=t[:, 0:n], in0=src[:, 0:n],
                                                                        in1=rope[:, ci, 0:n], op=ALU.mult),
                        [src, rope], [t])
                    t2 = T6()
                    dve(lambda e, t2=t2, br=br, ci=ci: e.tensor_tensor(out=t2[:, 0:n], in0=br[:, 0:n],
                                                                        in1=rope[:, ci + 1, 0:n], op=ALU.mult),
                        [br, rope], [t2])
                    dve(lambda e, t=t, t2=t2, dstf=dstf: e.tensor_tensor(out=dstf[:, 0:n], in0=t[:, 0:n],
                                                                          in1=t2[:, 0:n], op=ALU.add),
                        [t, t2], [dstf])
                    act(lambda e, dstf=dstf, dstb=dstb: e.activation(out=dstb[:, 0:n], in_=dstf[:, 0:n],
                                                                     func=AF.Copy), [dstf], [dstb])
                by = psb[7]
                if mode == "p":
                    bt = ps()
                    S.op("pe", lambda e, bt=bt: e.transpose(bt[:, 0:128], kfr[:, 0:128], ident[:, :]),
                         [kfr, ident], [bt])
                    act(lambda e, bt=bt: e.activation(out=ktm[:, :], in_=bt[:, 0:128], func=AF.Copy), [bt], [ktm])
                    bs = ps()
                    mm(bs[:, 0:128], kT[:, 0:128], qT[:, 0:128], True, True, [kT, qT], [bs])
                    dve(lambda e, bs=bs: e.tensor_tensor(out=sTm[:, :], in0=bs[:, 0:128], in1=maskT[:, :],
                                                         op=ALU.mult), [bs, maskT], [sTm])
                    mm(by[:, 0:128], v1[:, hh, :], sTm[:, :], True, False, [v1, sTm], [by])
                    mm(by[:, 0:128], Sret[l][hh][:, :], qfr[:, 0:128], False, True, [Sret[l][hh], qfr], [by])
                    bkv = ps()
                    mm(bkv[:, 0:128], ktm[:, :], v2[:, hh, :], True, True, [ktm, v2], [bkv])
                    St = Sret[l][hh]
                    dve(lambda e, St=St, bkv=bkv, hh=hh: e.scalar_tensor_tensor(
                        out=St[:, :], in0=St[:, :], scalar=float(GAM[hh] ** 128), in1=bkv[:, 0:128],
                        op0=ALU.mult, op1=ALU.add), [St, bkv], [St])
                    if last_tile:
                        S.dma("sp", dr["retp"][l, hh], St[:, :], reads=[St])
                    et = ept[hh % 2]
                    S.dma("sp", et[:, :], dr["epst"][:, hh, :], writes=[et])
                    eps_ap, eps_tl = et[:, 0:n], et
                else:
                    bt = ps()
                    S.op("pe", lambda e, bt=bt: e.transpose(bt[0:n, 0:128], kfr[:, 0:n], ident[:, :]),
                         [kfr, ident], [bt])
                    act(lambda e, bt=bt: e.activation(out=ktm32[0:n, :], in_=bt[0:n, 0:128], func=AF.Copy),
                        [bt], [ktm32])
                    for t in range(n):
                        s0 = S0[t % 3]
                        s1 = S1[t % 3]
                        S.dma("sp", s0[:, :], dr["sret"][l, tile_idx * SPC + t, hh], writes=[s0])
                        vm = Vm[t % 2]
                        dve(lambda e, vm=vm, hh=hh, t=t: e.tensor_scalar(
                            out=vm[:, :], in0=vs[0:16, hh * 128:(hh + 1) * 128], scalar1=eye16[:, t:t + 1],
                            scalar2=None, op0=ALU.mult), [vs, eye16], [vm])
                        bkv = ps()
                        mm(bkv[:, 0:128], ktm32[0:n, :], vm[:, :], True, True, [ktm32, vm], [bkv])
                        dve(lambda e, s0=s0, s1=s1, bkv=bkv, hh=hh: e.scalar_tensor_tensor(
                            out=s1[:, :], in0=s0[:, :], scalar=float(GAM[hh]), in1=bkv[:, 0:128],
                            op0=ALU.mult, op1=ALU.add), [s0, bkv], [s1])
                        S.dma("sp", dr["rets"][l, tile_idx * SPC + t, hh], s1[:, :], reads=[s1])
                        mm(by[:, t:t + 1], s1[:, :], qfr[:, t:t + 1], True, True, [s1, qfr], [by], inc=(t == n - 1))
                    eps_ap, eps_tl = epsc[:, 0:n], epsc
                act(lambda e, by=by: e.activation(out=yf[:, 0:n], in_=by[:, 0:n], func=AF.Copy), [by], [yf])
                yn = yn_t
                headnorm_fm(yf, yf[:, 0:n], n, ones, 1.0 / 128, eps_ap, eps_tl, yn, yn[:, 0:n])
                dve(lambda e, yn=yn, hh=hh: e.scalar_tensor_tensor(
                    out=yT[:, hh, 0:n], in0=yn[:, 0:n], scalar=par[:, 91 + hh:92 + hh], in1=SG[:, 0:n],
                    op0=ALU.mult, op1=ALU.mult), [yn, par, SG], [yT])
            sl = wload(dr["win_l"][l], 320, lambda s: s[:, 0:NKC * 320].rearrange("p (k c) -> p k c", k=NKC))
            wv = sl[:, 0:NKC * 320].rearrange("p (k c) -> p k c", k=NKC)
            for sbk, m in ((0, 128), (1, 128), (2, 64)):
                b = gemm_fm(sl, wv, sbk * 128, m, xnT, nrhs)
                tshift(b, m, n, mode, 24 + sbk, carry[l], 24 + sbk, LX[sbk][0:m, 0:n], LX[sbk])
            act(lambda e: e.activation(out=LX[0][0:64, 0:n], in_=LX[0][0:64, 0:n], func=AF.Tanh), [LX[0]], [LX[0]])
            act(lambda e: e.activation(out=LX[1][:, 0:n], in_=LX[1][:, 0:n], func=AF.Sigmoid), [LX[1]], [LX[1]])
            act(lambda e: e.activation(out=LX[2][0:32, 0:n], in_=LX[2][0:32, 0:n], func=AF.Sigmoid), [LX[2]], [LX[2]])
            R, KM, V, DEC, KK, B, VF, O = (RW[k] for k in ("R", "KM", "V", "DEC", "KK", "B", "VF", "O"))
            for fb in range(8):
                fc = slice(0, 128)
                lup = lupb[lup_rr[0] % 2]
                lup_rr[0] += 1
                for i in ((0, 2) if l > 0 else (0,)):
                    S.dma("sp", lup[i][:, :], dr["lup"][l, i, :, fb * 128:(fb + 1) * 128], writes=[lup[i]])
                sl = wload(dr["win_a"][l, 8 + fb], 384, lambda s: s[:, 0:NKC * 384].rearrange("p (k c) -> p k c", k=NKC))
                wv = sl[:, 0:NKC * 384].rearrange("p (k c) -> p k c", k=NKC)
                for j, arr in ((0, R), (1, None), (2, V)):
                    b = gemm_fm(sl, wv, j * 128, 128, xnT, nrhs)
                    if arr is None:
                        kraw = kraw_t
                        tshift(b, 128, n, mode, 8 * j + fb, carry[l], 8 * j + fb, kraw[:, 0:n], kraw)
                    else:
                        tshift(b, 128, n, mode, 8 * j + fb, carry[l], 8 * j + fb, arr[:, fb, 0:n], arr)
                b = ps()
                mm(b[:, 0:n], lup[0][0:64, fc], LX[0][0:64, 0:n], True, True, [lup[0], LX[0]], [b])
                t = SG if mode == "p" else T6()
                act(lambda e, b=b, t=t, fb=fb: e.activation(out=t[:, 0:n], in_=b[:, 0:n], func=AF.Sigmoid,
                                                            bias=par[:, 27 + fb:28 + fb]), [b, par], [t])
                if mode == "s":
                    act(lambda e, t=t, fb=fb: e.activation(out=DEC[:, fb, 0:n], in_=t[:, 0:n], func=AF.Exp,
                                                           scale=-C_DEC), [t], [DEC])
                b = ps()
                mm(b[:, 0:n], lup[0][64:128, fc], LX[0][64:128, 0:n], True, True, [lup[0], LX[0]], [b])
                At = At_t
                act(lambda e, b=b, fb=fb, At=At: e.activation(out=At[:, 0:n], in_=b[:, 0:n], func=AF.Sigmoid,
                                                              bias=par[:, 35 + fb:36 + fb]), [b, par], [At])
                if l == 0:
                    act(lambda e, fb=fb: e.activation(out=VF[:, fb, 0:n], in_=V[:, fb, 0:n], func=AF.Copy), [V], [VF])
                else:
                    b = ps()
                    mm(b[:, 0:n], lup[2][32:64, fc], LX[2][32:64, 0:n], True, True, [lup[2], LX[2]], [b])
                    vg = T6()
                    act(lambda e, b=b, vg=vg, fb=fb: e.activation(out=vg[:, 0:n], in_=b[:, 0:n], func=AF.Sigmoid,
                                                                  bias=par[:, 43 + fb:44 + fb]), [b, par], [vg])
                    dd = T6()
                    dve(lambda e, dd=dd, fb=fb: e.tensor_tensor(out=dd[:, 0:n], in0=VF[:, fb, 0:n], in1=V[:, fb, 0:n],
                                                                op=ALU.subtract), [VF, V], [dd])
                    dve(lambda e, dd=dd, vg=vg: e.tensor_tensor(out=dd[:, 0:n], in0=dd[:, 0:n], in1=vg[:, 0:n],
                                                                op=ALU.mult), [dd, vg], [dd])
                    dve(lambda e, dd=dd, fb=fb: e.tensor_tensor(out=V[:, fb, 0:n], in0=V[:, fb, 0:n], in1=dd[:, 0:n],
                                                                op=ALU.add), [V, dd], [V])
                kx = T6()
                dve(lambda e, kx=kx, kraw=kraw, fb=fb: e.tensor_scalar(out=kx[:, 0:n], in0=kraw[:, 0:n],
                                                                       scalar1=par[:, 51 + fb:52 + fb], scalar2=None,
                                                                       op0=ALU.mult), [kraw, par], [kx])
                sq = T6()
                act(lambda e, sq=sq, kx=kx: e.activation(out=sq[:, 0:n], in_=kx[:, 0:n], func=AF.Square), [kx], [sq])
                b = ps()
                mm(b[:, 0:n], onesblk[:, :], sq[:, 0:n], True, True, [onesblk, sq], [b])
                rn = T6()
                dve(lambda e, rn=rn, b=b: e.tensor_scalar(out=rn[:, 0:n], in0=b[:, 0:n], scalar1=1e-24, scalar2=None,
                                                          op0=ALU.add), [b], [rn])
                act(lambda e, rn=rn: e.activation(out=rn[:, 0:n], in_=rn[:, 0:n], func=AF.Sqrt), [rn], [rn])
                dve(lambda e, rn=rn: e.reciprocal(out=rn[:, 0:n], in_=rn[:, 0:n]), [rn], [rn])
                dve(lambda e, kx=kx, rn=rn, fb=fb: e.tensor_tensor(out=KK[:, fb, 0:n], in0=kx[:, 0:n], in1=rn[:, 0:n],
                                                                   op=ALU.mult), [kx, rn], [KK])
                t = T6()
                dve(lambda e, t=t, fb=fb, At=At: e.tensor_scalar(out=t[:, 0:n], in0=At[:, 0:n],
                                                                 scalar1=par[:, 59 + fb:60 + fb], scalar2=omka[:, fb:fb + 1],
                                                                 op0=ALU.mult, op1=ALU.add), [At, par, omka], [t])
                dve(lambda e, t=t, kraw=kraw, fb=fb: e.tensor_tensor(out=KM[:, fb, 0:n], in0=kraw[:, 0:n],
                                                                     in1=t[:, 0:n], op=ALU.mult), [kraw, t], [KM])
                dve(lambda e, fb=fb, At=At: e.tensor_tensor(out=B[:, fb, 0:n], in0=KK[:, fb, 0:n], in1=At[:, 0:n],
                                                            op=ALU.mult), [KK, At], [B])
                if mode == "p":
                    chunk_pair(l, fb)
            if mode == "p" and last_tile:
                so = T6()
                for fb in range(8):
                    for h2 in range(2):
                        r_ = slice(h2 * 64, (h2 + 1) * 64)
                        S.op("pool", lambda e, r_=r_, fb=fb: e.tensor_copy(out=Tbd[r_, r_], in_=Tst[l][r_, fb, :]),
                             [Tst[l]], [Tbd])
                    bt = ps()
                    S.op("pe", lambda e, bt=bt: e.transpose(bt[:, 0:128], Tbd[:, :], ident[:, :]), [Tbd, ident], [bt])
                    for h2 in range(2):
                        r_ = slice(h2 * 64, (h2 + 1) * 64)
                        act(lambda e, r_=r_, fb=fb, bt=bt: e.activation(out=so[r_, fb * 64:(fb + 1) * 64],
                                                                      in_=bt[r_, r_], func=AF.Copy), [bt], [so])
                S.dma("sp", dr["rwp"][l].rearrange("(f h) v k -> (h v) f k", h=2),
                      so[:, :].rearrange("p (a c) -> p a c", a=8), reads=[so])
            for t in range(n if mode == "s" else 0):
                if True:
                    ST = STs[t % 2]
                    S.dma("sp", ST[:, :, :], dr["srw"][l, tile_idx * SPC + t].rearrange("(f h) v k -> (h v) f k", h=2), writes=[ST])
                bc = []
                for i, arr in enumerate((KK, DEC, B, KM, R)):
                    dg = DG[dg_rr[0] % 3]
                    dg_rr[0] += 1
                    S.op("pool", lambda e, dg=dg, arr=arr, t=t: e.tensor_tensor(
                        out=dg[:, :, :], in0=arr[:, :, t:t + 1].to_broadcast([128, 8, 64]),
                        in1=i64s[:, :].unsqueeze(1).to_broadcast([128, 8, 64]), op=ALU.mult),
                        [arr, i64s], [dg])
                    b = ps()
                    mm(b[:, :], onesblk[:, :], dg[:, :, :].rearrange("p a c -> p (a c)"), True, True,
                       [onesblk, dg], [b])
                    bc.append(b)
                bKK, bDEC, bB, bKM, bR = bc
                ta = T6(); tb = T6(); tc = T6()

                def v3(x):
                    return x[:, :].rearrange("p (a c) -> p a c", a=8)

                dve(lambda e, ta=ta, ST=ST, b=bKK: e.tensor_tensor(out=v3(ta), in0=ST[:, :, :], in1=v3(b), op=ALU.mult),
                    [ST, bKK], [ta])
                dve(lambda e, ta=ta: e.tensor_reduce(out=sa[:, :], in_=v3(ta), axis=AX.X, op=ALU.add), [ta], [sa])
                dve(lambda e, tb=tb, ST=ST, b=bDEC: e.tensor_tensor(out=v3(tb), in0=ST[:, :, :], in1=v3(b), op=ALU.mult),
                    [ST, bDEC], [tb])
                dve(lambda e, tc=tc, b=bB: e.tensor_tensor(out=v3(tc), in0=v3(b),
                                                           in1=sa[:, :].unsqueeze(2).to_broadcast([128, 8, 64]),
                                                           op=ALU.mult), [bB, sa], [tc])
                dve(lambda e, tb=tb, tc=tc: e.tensor_tensor(out=tb[:, :], in0=tb[:, :], in1=tc[:, :], op=ALU.subtract),
                    [tb, tc], [tb])
                dve(lambda e, tc=tc, b=bKM, t=t: e.tensor_tensor(out=v3(tc), in0=v3(b),
                                                                 in1=V[:, :, t:t + 1].to_broadcast([128, 8, 64]),
                                                                 op=ALU.mult), [bKM, V], [tc])
                dve(lambda e, ST=ST, tb=tb, tc=tc: e.tensor_tensor(out=ST[:, :, :], in0=v3(tb), in1=v3(tc), op=ALU.add),
                    [tb, tc], [ST])
                dve(lambda e, ta=ta, ST=ST, b=bR: e.tensor_tensor(out=v3(ta), in0=ST[:, :, :], in1=v3(b), op=ALU.mult),
                    [ST, bR], [ta])
                dve(lambda e, ta=ta, t=t: e.tensor_reduce(out=O[:, :, t:t + 1], in_=v3(ta), axis=AX.X, op=ALU.add),
                    [ta], [O])
                if mode == "s":
                    S.dma("sp", dr["rws"][l, tile_idx * SPC + t].rearrange("(f h) v k -> (h v) f k", h=2), ST[:, :, :], reads=[ST])
            for fb in range(8):
                yn = yn_t
                headnorm_fm(O, O[:, fb, 0:n], n, onesblk, 1.0 / 64, epsw[:, 0:n], epsw, yn, yn[:, 0:n])
                dve(lambda e, yn=yn, fb=fb: e.tensor_scalar(out=yn[:, 0:n], in0=yn[:, 0:n],
                                                            scalar1=par[:, 75 + fb:76 + fb],
                                                            scalar2=par[:, 83 + fb:84 + fb], op0=ALU.mult, op1=ALU.add),
                    [yn, par], [yn])
                fc = slice(0, 128)
                lup = lupb[lup_rr[0] % 2]
                lup_rr[0] += 1
                for i in (1, 2):
                    S.dma("sp", lup[i][:, :], dr["lup"][l, i, :, fb * 128:(fb + 1) * 128], writes=[lup[i]])
                t = T6()
                dve(lambda e, t=t, fb=fb: e.scalar_tensor_tensor(out=t[:, 0:n], in0=R[:, fb, 0:n],
                                                                 scalar=par[:, 67 + fb:68 + fb], in1=KM[:, fb, 0:n],
                                                                 op0=ALU.mult, op1=ALU.mult), [R, par, KM], [t])
                b = ps()
                mm(b[:, 0:n], onesblk[:, :], t[:, 0:n], True, True, [onesblk, t], [b])
                bon = T6()
                dve(lambda e, b=b, fb=fb, bon=bon: e.tensor_tensor(out=bon[:, 0:n], in0=b[:, 0:n], in1=V[:, fb, 0:n],
                                                                   op=ALU.mult), [b, V], [bon])
                dve(lambda e, yn=yn, bon=bon: e.tensor_tensor(out=yn[:, 0:n], in0=yn[:, 0:n], in1=bon[:, 0:n],
                                                              op=ALU.add), [yn, bon], [yn])
                bg = ps()
                mm(bg[:, 0:n], lup[1][:, fc], LX[1][:, 0:n], True, False, [lup[1], LX[1]], [bg])
                mm(bg[:, 0:n], lup[2][0:32, fc], LX[2][0:32, 0:n], False, True, [lup[2], LX[2]], [bg])
                dve(lambda e, yn=yn, fb=fb, bg=bg: e.tensor_tensor(out=yT[:, 8 + fb, 0:n], in0=yn[:, 0:n], in1=bg[:, 0:n],
                                                                   op=ALU.mult), [yn, bg], [yT])
            for cb in range(4):
                sl = wload(dr["wout"][l, cb], 512, lambda s: s[:, :].rearrange("p (k c) -> p k c", k=NKC))
                wv = sl[:, :].rearrange("p (k c) -> p k c", k=NKC)
                b = ps()
                for kc in range(NKC):
                    mm(b[0:n, :], yT[:, kc, 0:n], wv[:, kc, :], kc == 0, kc == NKC - 1, [sl, yT], [b])
                dve(lambda e, b=b, cb=cb: e.tensor_tensor(out=h[0:n, cb * 512:(cb + 1) * 512],
                                                          in0=h[0:n, cb * 512:(cb + 1) * 512], in1=b[0:n, :],
                                                          op=ALU.add), [h, b], [h])
            norm(n, 2 * l + 1)
            to_fm(xn, n, xnT, 0)
            for g in range(NG):
                slg = wload(dr["wg"][l, g], 512, lambda s: s[:, :].rearrange("p (k c) -> p k c", k=NKC))
                slu = wload(dr["wu"][l, g], 512, lambda s: s[:, :].rearrange("p (k c) -> p k c", k=NKC))
                sld = wload(dr["wd"][l, g], 512, lambda s: s[:, :].rearrange("p (k c) -> p k c", k=4))
                wgv = slg[:, :].rearrange("p (k c) -> p k c", k=NKC)
                wuv = slu[:, :].rearrange("p (k c) -> p k c", k=NKC)
                wdv = sld[:, :].rearrange("p (k c) -> p k c", k=4)
                aT = actT[g % 2]
                for j in range(4):
                    b1 = gemm_fm(slg, wgv, j * 128, 128, xnT, n)
                    b2 = gemm_fm(slu, wuv, j * 128, 128, xnT, n)
                    act(lambda e, b1=b1: e.activation(out=sgt[:, 0:n], in_=b1[:, 0:n], func=AF.Silu), [b1], [sgt])
                    dve(lambda e, b2=b2, j=j, aT=aT: e.tensor_tensor(out=aT[:, j, 0:n], in0=sgt[:, 0:n],
                                                                      in1=b2[:, 0:n], op=ALU.mult), [sgt, b2], [aT])
                for cb in range(4):
                    b = ps()
                    for kc in range(4):
                        mm(b[0:n, :], aT[:, kc, 0:n], wdv[:, kc, cb * 512:(cb + 1) * 512], kc == 0, kc == 3,
                           [sld, aT], [b])
                    dve(lambda e, b=b, cb=cb: e.tensor_tensor(out=h[0:n, cb * 512:(cb + 1) * 512],
                                                              in0=h[0:n, cb * 512:(cb + 1) * 512], in1=b[0:n, :],
                                                              op=ALU.add), [h, b], [h])

        for ti in range(NT):
            S.dma("sp", h[:, :], dr["xp"][ti * TT:(ti + 1) * TT, :], writes=[h])
            S.dma("sp", rope[:, :, :], dr["rope_p"][:, :, ti * TT:(ti + 1) * TT], writes=[rope])
            for l in range(L):
                layer(l, "p", TT, ti, ti == NT - 1)
            norm(TT, 2 * L)
            S.dma("sp", dr["yp"][ti * TT:(ti + 1) * TT, :], xn[:, :], reads=[xn])
        if with_sample:
            for sg in range(NSG):
                S.dma("sp", h[0:SPC, :], dr["xs"][sg * SPC:(sg + 1) * SPC, :], writes=[h])
                S.dma("sp", rope[:, :, 0:SPC], dr["rope_s"], writes=[rope])
                for l in range(L):
                    layer(l, "s", SPC, sg, False)
                norm(SPC, 2 * L)
                S.dma("sp", dr["ys"][sg * SPC:(sg + 1) * SPC, :], xn[0:SPC, :], reads=[xn])
        S.finish()

        with nc.Block() as block:
            @block.tensor
            def _(e):
                for f in S.E["pe"].q:
                    f(e)

            @block.scalar
            def _(e):
                for f in S.E["act"].q:
                    f(e)

            @block.vector
            def _(e):
                for f in S.E["dve"].q:
                    f(e)

            @block.gpsimd
            def _(e):
                for f in S.E["pool"].q:
                    f(e)

            @block.sync
            def _(e):
                for f in S.E["sp"].q:
                    f(e)
    return nc


def run(inp, L, T, B, with_sample=True):
    import time as _t
    _t0 = _t.time()
    hp = host_prep(inp, L)
    cs = host_consts(T)
    print("host_prep s", _t.time() - _t0, flush=True)
    _t0 = _t.time()
    nc = build(L, T, with_sample)
    print("build s", _t.time() - _t0, flush=True)
    _t0 = _t.time()
    in_maps = []
    for c in range(NCORE):
        m = dict(hp)
        m.update(cs)
        b = c % B
        m["xp"] = np.ascontiguousarray(inp["x_prompt"][b, :T])
        m["xs"] = np.ascontiguousarray(inp["x_sample"][c * SPCORE:(c + 1) * SPCORE, 0])
        m["sret"] = np.ascontiguousarray(inp["state_ret"][:L, c * SPCORE:(c + 1) * SPCORE])
        m["srw"] = np.ascontiguousarray(inp["state_rwkv"][:L, c * SPCORE:(c + 1) * SPCORE])
        m["ssh"] = np.ascontiguousarray(inp["state_shift"][:L, c * SPCORE:(c + 1) * SPCORE])
        in_maps.append(m)
    res = run_bass_kernel_spmd(nc, in_maps, core_ids=list(range(NCORE)))
    print("spmd s", _t.time() - _t0, flush=True)
    r = res.results
    yp = np.stack([r[b]["yp"] for b in range(B)])
    ys = np.concatenate([r[c]["ys"] for c in range(NCORE)])[:, None, :]
    retp = np.stack([r[b]["retp"] for b in range(B)], 1)
    rwp = np.stack([r[b]["rwp"] for b in range(B)], 1)
    shp = np.stack([r[b]["shp"] for b in range(B)], 1)
    rets = np.concatenate([r[c]["rets"] for c in range(NCORE)], 1)
    rws = np.concatenate([r[c]["rws"] for c in range(NCORE)], 1)
    shs = np.concatenate([r[c]["shs"] for c in range(NCORE)], 1)
    return (yp, ys, retp, rwp, shp, rets, rws, shs)


def kernel(**inputs):
    inp = {k: np.asarray(v) for k, v in inputs.items()}
    L = inp["w_in"].shape[0]
    B, T = inp["x_prompt"].shape[0], inp["x_prompt"].shape[1]
    return run(inp, L, T, B)
```

```python
import contextlib
import numpy as np
import concourse.bass as bass
import concourse.mybir as mybir
from concourse.bass_utils import run_bass_kernel_spmd

F32 = mybir.dt.float32
BF16 = mybir.dt.bfloat16
AF = mybir.ActivationFunctionType
ALU = mybir.AluOpType
AX = mybir.AxisListType

D = 2048
NKC = 16
DFF = 5632
NG = 11
RETW = 1024
SPC = 16
NSG = 2
NCORE = 4
SPCORE = SPC * NSG
TT = 128
NPAR = 104
C_DEC = float(np.exp(-0.5))
GAM = [1.0 - 2.0 ** (-5.0 - h) for h in range(8)]


class Sem:
    def __init__(self, h):
        self.h = h
        self.val = 0


class Eng:
    def __init__(self, name, sem):
        self.name = name
        self.sem = sem
        self.q = []
        self.seen = {}


class Tl:
    def __init__(self, t, name):
        self.t = t
        self.name = name
        self.w = None
        self.r = {}

    def __getitem__(self, k):
        return self.t[k]


class Sched:
    def __init__(self, nc, stack, ndma_sp=20, ndma_pool=8):
        self.nc = nc
        self.stack = stack
        self.E = {}
        for n in ("pe", "act", "dve", "pool", "sp"):
            self.E[n] = Eng(n, Sem(stack.enter_context(nc.semaphore("s_" + n))))
        self.dsem = {
            "sp": [Sem(stack.enter_context(nc.semaphore(f"dsp{i}"))) for i in range(ndma_sp)],
            "pool": [Sem(stack.enter_context(nc.semaphore(f"dpl{i}"))) for i in range(ndma_pool)],
        }
        self.dcnt = {"sp": 0, "pool": 0}
        self.nalloc = 0
        self.ps_rr = 0

    def sb(self, name, shape, dt=F32):
        t = self.stack.enter_context(self.nc.sbuf_tensor("sb_" + name, list(shape), dt))
        return Tl(t, name)

    def _wait(self, E, ev):
        sem, val = ev
        if E.seen.get(id(sem), 0) >= val:
            return
        E.seen[id(sem)] = val
        E.q.append(lambda e, h=sem.h, v=val: e.wait_ge(h, v))

    def _deps(self, E, reads, writes, selfsync):
        evs = []
        for b in reads:
            if b.w is not None:
                evs.append(b.w)
        for b in writes:
            if b.w is not None:
                evs.append(b.w)
            evs.extend(b.r.values())
        for ev in evs:
            if (not selfsync) and ev[0] is E.sem:
                continue
            self._wait(E, ev)

    def _reg(self, me, reads, writes):
        for b in reads:
            b.r[id(me[0])] = me
        for b in writes:
            b.w = me
            b.r = {}

    def op(self, en, fn, reads=(), writes=(), inc=True):
        E = self.E[en]
        self._deps(E, reads, writes, selfsync=(en != "pe"))
        if inc:
            E.sem.val += 1
            me = (E.sem, E.sem.val)
            E.q.append(lambda e, f=fn, h=E.sem.h: f(e).then_inc(h, 1))
        else:
            me = (E.sem, E.sem.val + 1)
            E.q.append(lambda e, f=fn: f(e))
        self._reg(me, reads, writes)

    def dma(self, qn, out, in_, reads=(), writes=()):
        E = self.E[qn]
        pool = self.dsem[qn]
        sem = pool[self.dcnt[qn] % len(pool)]
        self.dcnt[qn] += 1
        self._deps(E, reads, writes, selfsync=True)
        if sem.val > 0:
            self._wait(E, (sem, sem.val))
        sem.val += 16
        me = (sem, sem.val)
        E.q.append(lambda e, o=out, i=in_, h=sem.h: e.dma_start(out=o, in_=i).then_inc(h, 16))
        self._reg(me, reads, writes)

    def finish(self):
        E = self.E["sp"]
        for qn in ("sp", "pool"):
            for sem in self.dsem[qn]:
                if sem.val > 0:
                    self._wait(E, (sem, sem.val))
        for n in ("pe", "act", "dve", "pool"):
            s = self.E[n].sem
            if s.val > 0:
                self._wait(E, (s, s.val))


def _blk(w, cols):
    x = w[:, cols]
    return np.ascontiguousarray(x.reshape(NKC, 128, x.shape[1]).transpose(1, 0, 2))


def host_prep(inp, L):
    f32 = np.float32
    out = {}
    win_a = np.empty((L, 16, 128, NKC, 384), f32)
    win_v = np.empty((L, 2, 128, NKC, 512), f32)
    win_l = np.empty((L, 128, NKC, 320), f32)
    wout = np.empty((L, 4, 128, NKC, 512), f32)
    wg = np.empty((L, NG, 128, NKC, 512), f32)
    wu = np.empty((L, NG, 128, NKC, 512), f32)
    wd = np.empty((L, NG, 128, 4, D), f32)
    par = np.zeros((L, 128, NPAR), f32)
    lup = np.zeros((L, 3, 128, 1024), f32)
    gvec = np.zeros((2 * L + 1, D), f32)
    ar = np.arange(128)
    for l in range(L):
        w = inp["w_in"][l]
        if l > 0:
            w = np.concatenate([w, inp["w_in_vres"][l - 1]], axis=1)
        else:
            w = np.concatenate([w, np.zeros((D, 32), f32)], axis=1)
        for h in range(8):
            cols = np.concatenate([h * 128 + ar, 1024 + h * 128 + ar, 3072 + h * 128 + ar])
            win_a[l, h] = _blk(w, cols)
            cols = 4096 + np.concatenate([h * 128 + ar, 1024 + h * 128 + ar, 2048 + h * 128 + ar])
            win_a[l, 8 + h] = _blk(w, cols)
        for cb in range(2):
            win_v[l, cb] = _blk(w, 2048 + cb * 512 + np.arange(512))
        win_l[l] = _blk(w, 7168 + np.arange(320))
        for cb in range(4):
            wout[l, cb] = _blk(inp["w_out"][l], cb * 512 + np.arange(512))
        for g in range(NG):
            wg[l, g] = _blk(inp["w_gate"][l], g * 512 + np.arange(512))
            wu[l, g] = _blk(inp["w_up"][l], g * 512 + np.arange(512))
            wd[l, g] = inp["w_down"][l][g * 512:(g + 1) * 512].reshape(4, 128, D).transpose(1, 0, 2)
        mu = inp["mu_shift"][l]
        if l > 0:
            mu = np.concatenate([mu, inp["mu_shift_vres"][l - 1]])
        else:
            mu = np.concatenate([mu, np.zeros(32, f32)])
        for fb in range(8):
            par[l, :, fb] = mu[fb * 128:(fb + 1) * 128]
            par[l, :, 8 + fb] = mu[1024 + fb * 128:1024 + (fb + 1) * 128]
            par[l, :, 16 + fb] = mu[2048 + fb * 128:2048 + (fb + 1) * 128]
        par[l, :, 24] = mu[3072:3200]
        par[l, :, 25] = mu[3200:3328]
        par[l, :64, 26] = mu[3328:3392]

        def pk(v, c0):
            par[l, :, c0:c0 + 8] = v.reshape(8, 128).T

        pk(inp["rwkv_w0"][l], 27)
        pk(inp["rwkv_a0"][l], 35)
        if l > 0:
            pk(inp["rwkv_v0"][l - 1], 43)
        pk(inp["rwkv_k_k"][l], 51)
        pk(inp["rwkv_k_a"][l], 59)
        pk(inp["rwkv_r_k"][l].reshape(-1), 67)
        pk(inp["rwkv_ln_w"][l], 75)
        pk(inp["rwkv_ln_b"][l], 83)
        pk(inp["ret_ln_w"][l], 91)
        lup[l, 0, :64] = inp["rwkv_w_up"][l]
        lup[l, 0, 64:] = inp["rwkv_a_up"][l]
        lup[l, 1] = inp["rwkv_g_up"][l][:128]
        lup[l, 2, :32] = inp["rwkv_g_up"][l][128:160]
        if l > 0:
            lup[l, 2, 32:64] = inp["rwkv_v_up"][l - 1]
        gvec[2 * l] = inp["g_attn"][l]
        gvec[2 * l + 1] = inp["g_ffn"][l]
    gvec[2 * L] = inp["g_final"]
    out.update(win_a=win_a, win_v=win_v, win_l=win_l, wout=wout, wg=wg, wu=wu, wd=wd,
               par=par, lup=lup, gvec=gvec)
    return out


def host_consts(T):
    f32 = np.float32
    c = {}
    c["ident"] = np.eye(128, dtype=f32)
    ob = np.zeros((128, 128), f32)
    ob[:64, :64] = 1
    ob[64:, 64:] = 1
    c["onesblk"] = ob
    c["ones"] = np.ones((128, 128), f32)
    c["i64s"] = np.concatenate([np.eye(64, dtype=f32), np.eye(64, dtype=f32)], axis=0)
    jj = np.arange(128)
    c["maskT"] = (jj[:, None] <= jj[None, :]).astype(f32)
    rot = np.zeros((128, 128), f32)
    for dp in range(64):
        rot[dp + 64, dp] = -1.0
        rot[dp, dp + 64] = 1.0
    c["rotT"] = rot
    half = 64
    inv = (np.float32(10000.0) ** (-(np.arange(half, dtype=f32)) / np.float32(half))).astype(f32)
    def tabs(pos):
        ang = (pos[:, None].astype(f32) * inv[None, :]).astype(f32)
        cs = np.cos(ang).astype(f32).T
        sn = np.sin(ang).astype(f32).T
        return np.concatenate([cs, cs], 0), np.concatenate([sn, sn], 0)
    cs, sn = tabs(np.arange(T, dtype=f32))
    sc = np.float32(128.0 ** -0.5)
    c["rope_p"] = np.ascontiguousarray(np.stack([cs, sn, cs * sc, sn * sc], 1))
    cs, sn = tabs(np.full(SPC, 16384.0, dtype=f32))
    c["rope_s"] = np.ascontiguousarray(np.stack([cs, sn, cs * sc, sn * sc], 1))
    lg = np.log1p(-np.exp2(-5.0 - np.arange(8, dtype=np.float64)))
    j = np.arange(128, dtype=np.float64)
    g1 = np.exp(lg[None, :] * (-j[:, None] - 1.0))
    g2 = np.exp(lg[None, :] * (127.0 - j[:, None]))
    c["gtab"] = np.ascontiguousarray(np.stack([g1, g2], 1).astype(f32))
    eps = 1e-5 / np.exp(lg[:, None] * 2.0 * (j[None, :] + 1.0))
    c["epst"] = np.ascontiguousarray(np.broadcast_to(eps[None].astype(f32), (128, 8, 128)))
    c["eye16"] = np.eye(16, dtype=f32)
    a64 = np.arange(128) % 64
    blk = (np.arange(128)[:, None] // 64) == (np.arange(128)[None, :] // 64)
    mST = (a64[:, None] < a64[None, :]).astype(f32)
    mIT = (a64[:, None] <= a64[None, :]).astype(f32)
    mSL = (a64[:, None] > a64[None, :]).astype(f32)
    UT = (blk & (a64[:, None] <= a64[None, :])).astype(f32)
    c["cm"] = np.ascontiguousarray(np.stack([-mST, -mSL, mST, mIT, UT], 1))
    return c


def build(L, T, with_sample=True):
    NT = T // TT
    nc = bass.Bass("TRN2", target_bir_lowering=False)
    dr = {}

    def din(name, shape):
        dr[name] = nc.dram_tensor(name, list(shape), F32, kind="ExternalInput").ap()
        return dr[name]

    def dout(name, shape):
        dr[name] = nc.dram_tensor(name, list(shape), F32, kind="ExternalOutput").ap()
        return dr[name]

    din("xp", (T, D)); din("xs", (SPCORE, D))
    din("sret", (L, SPCORE, 8, 128, 128)); din("srw", (L, SPCORE, 16, 64, 64)); din("ssh", (L, SPCORE, D))
    din("win_a", (L, 16, 128, NKC, 384)); din("win_v", (L, 2, 128, NKC, 512)); din("win_l", (L, 128, NKC, 320))
    din("wout", (L, 4, 128, NKC, 512)); din("wg", (L, NG, 128, NKC, 512)); din("wu", (L, NG, 128, NKC, 512))
    din("wd", (L, NG, 128, 4, D)); din("par", (L, 128, NPAR)); din("lup", (L, 3, 128, 1024))
    din("gvec", (2 * L + 1, D))
    for n, s in (("ident", (128, 128)), ("onesblk", (128, 128)), ("ones", (128, 128)), ("i64s", (128, 64)),
                 ("maskT", (128, 128)), ("rotT", (128, 128)), ("rope_p", (128, 4, T)), ("rope_s", (128, 4, SPC)),
                 ("gtab", (128, 2, 8)), ("epst", (128, 8, 128)), ("eye16", (16, 16)), ("cm", (128, 5, 128))):
        din(n, s)
    dout("yp", (T, D)); dout("ys", (SPCORE, D))
    dout("retp", (L, 8, 128, 128)); dout("rwp", (L, 16, 64, 64)); dout("shp", (L, D))
    dout("rets", (L, SPCORE, 8, 128, 128)); dout("rws", (L, SPCORE, 16, 64, 64)); dout("shs", (L, SPCORE, D))

    scr = {}
    scr_tl = {}
    for key, shp in (("win_a", (L, 16, 128, NKC, 384)), ("win_v", (L, 2, 128, NKC, 512)), ("win_l", (L, 128, NKC, 320)),
                     ("wout", (L, 4, 128, NKC, 512)), ("wg", (L, NG, 128, NKC, 512)), ("wu", (L, NG, 128, NKC, 512)),
                     ("wd", (L, NG, 128, 4, D))):
        scr[key] = nc.dram_tensor("scr_" + key, list(shp), BF16, kind="Internal").ap()
        scr_tl[key] = Tl(None, "scr_" + key)
    first_pass = [True]

    with contextlib.ExitStack() as stack:
        S = Sched(nc, stack)
        sb = S.sb
        ident = sb("ident", (128, 128)); onesblk = sb("onesblk", (128, 128)); ones = sb("ones", (128, 128))
        i64s = sb("i64s", (128, 64)); maskT = sb("maskT", (128, 128)); rotT = sb("rotT", (128, 128))
        gtab = sb("gtab", (128, 2, 8)); ept = [sb(f"ept{i}", (128, 128)) for i in range(2)]; eye16 = sb("eye16", (16, 16))
        cm = sb("cm", (128, 5, 128))
        epsc = sb("epsc", (128, 128)); epsw = sb("epsw", (128, 128))
        rope = sb("rope", (128, 4, TT))
        for tl, n in ((ident, "ident"), (onesblk, "onesblk"), (ones, "ones"), (i64s, "i64s"), (maskT, "maskT"),
                      (rotT, "rotT"), (gtab, "gtab"), (cm, "cm"), (eye16, "eye16")):
            S.dma("sp", tl[:], dr[n], writes=[tl])
        S.op("dve", lambda e: e.memset(epsc[:], 1e-5), writes=[epsc])
        S.op("dve", lambda e: e.memset(epsw[:], 64e-5), writes=[epsw])
        par = sb("par", (128, NPAR)); omka = sb("omka", (128, 8))
        lupb = [[sb(f"lup{i}_{j}", (128, 128)) for i in range(3)] for j in range(2)]
        lup_rr = [0]
        gb = sb("gb", (128, D))
        NSLOT = 3
        slots = [sb(f"wslot{i}", (128, NKC * 512), BF16) for i in range(NSLOT)]
        slot_rr = [0]
        h = sb("h", (128, D)); xn = sb("xn", (128, D))
        xnT = sb("xnT", (128, NKC, TT), BF16); yT = sb("yT", (128, NKC, TT), BF16)
        ss = sb("ss", (128, 1)); rstd = sb("rstd", (128, 1))
        psb = [Tl(stack.enter_context(nc.psum_tensor(f"ps{i}", [128, 512], F32)), f"ps{i}") for i in range(8)]

        def ps():
            b = psb[S.ps_rr % 7]
            S.ps_rr += 1
            return b

        Sret = [[sb(f"Sret{l}_{hh}", (128, 128)) for hh in range(8)] for l in range(L)]
        Tst = [sb(f"Tst{l}", (128, 8, 64)) for l in range(L)]
        Tbd = sb("Tbd", (128, 128))
        carry = [sb(f"carry{l}", (128, 32)) for l in range(L)]
        for l in range(L):
            for hh in range(8):
                S.op("dve", lambda e, t=Sret[l][hh]: e.memset(t[:], 0.0), writes=[Sret[l][hh]])
            S.op("dve", lambda e, t=Tst[l]: e.memset(t[:], 0.0), writes=[Tst[l]])
            S.op("dve", lambda e, t=carry[l]: e.memset(t[:], 0.0), writes=[carry[l]])
        RW = {n: sb("rw_" + n, (128, 8, TT)) for n in ("R", "KM", "V", "DEC", "KK", "B", "VF", "O")}
        LX = [sb(f"LX{i}", (128, TT)) for i in range(3)]
        P = [sb(f"P{i}", (128, TT + 1)) for i in range(2)]
        p_rr = [0]
        tmp = [sb(f"tmp{i}", (128, 512)) for i in range(6)]
        t_rr = [0]
        DG = [sb(f"DG{i}", (128, 8, 64)) for i in range(3)]
        dg_rr = [0]
        sa = sb("sa", (128, 8))
        kraw_t = sb("kraw", (128, TT)); At_t = sb("At", (128, TT)); yn_t = sb("yn", (128, TT))
        v1 = sb("v1", (128, 8, 128), BF16); v2 = sb("v2", (128, 8, 128), BF16); vs = sb("vs", (16, 1024))
        qf = sb("qf", (128, TT)); kf = sb("kf", (128, TT)); SG = sb("SG", (128, TT))
        qT = sb("qT", (128, TT), BF16); kT = sb("kT", (128, TT), BF16); ktm = sb("ktm", (128, 128), BF16)
        ktm32 = sb("ktm32", (16, 128)); kfr = sb("kfr", (128, TT)); qfr = sb("qfr", (128, TT))
        sTm = sb("sTm", (128, 128), BF16); yf = sb("yf", (128, TT))
        Vm = [sb(f"Vm{i}", (16, 128)) for i in range(2)]
        S0 = [sb(f"S0_{i}", (128, 128)) for i in range(3)]
        S1 = [sb(f"S1_{i}", (128, 128)) for i in range(3)]
        STs = [sb(f"STs{i}", (128, 8, 64)) for i in range(2)]
        actT = [sb(f"actT{i}", (128, 4, TT), BF16) for i in range(2)]
        sgt = sb("sgt", (128, TT))

        def T6():
            t = tmp[t_rr[0] % 6]
            t_rr[0] += 1
            return t

        def wload(key, idx, ncols_total, view):
            sl = slots[slot_rr[0] % NSLOT]
            slot_rr[0] += 1
            if first_pass[0]:
                S.dma("pool", view(sl), dr[key][idx], writes=[sl])
                S.dma("sp", scr[key][idx], view(sl), reads=[sl], writes=[scr_tl[key]])
            else:
                S.dma("pool", view(sl), scr[key][idx], reads=[scr_tl[key]], writes=[sl])
            return sl

        def act(fn, reads, writes):
            S.op("act", fn, reads, writes)

        def dve(fn, reads, writes):
            S.op("dve", fn, reads, writes)

        def mm(out, lhsT, rhs, start, stop, reads, writes, inc=None):
            S.op("pe", lambda e: e.matmul(out, lhsT, rhs, start=start, stop=stop), reads, writes,
                 inc=(stop if inc is None else inc))

        def norm(n, gidx, shift_out=None):
            S.dma("sp", gb[0:n, :], dr["gvec"][gidx:gidx + 1, :].to_broadcast([n, D]), writes=[gb])
            dve(lambda e: e.memset(ss[0:n, :], 0.0), [], [ss])
            act(lambda e: e.activation(out=xn[0:n, :], in_=h[0:n, :], func=AF.Square, accum_out=ss[0:n, :]),
                [h, ss], [xn, ss])
            dve(lambda e: e.tensor_scalar(out=rstd[0:n, :], in0=ss[0:n, :], scalar1=1.0 / D, scalar2=1e-6,
                                          op0=ALU.mult, op1=ALU.add), [ss], [rstd])
            act(lambda e: e.activation(out=rstd[0:n, :], in_=rstd[0:n, :], func=AF.Sqrt), [rstd], [rstd])
            dve(lambda e: e.reciprocal(out=rstd[0:n, :], in_=rstd[0:n, :]), [rstd], [rstd])
            dve(lambda e: e.scalar_tensor_tensor(out=xn[0:n, :], in0=h[0:n, :], scalar=rstd[0:n, 0:1],
                                                 in1=gb[0:n, :], op0=ALU.mult, op1=ALU.mult),
                [h, rstd, gb], [xn])
            if shift_out is not None:
                S.dma("sp", shift_out, xn[(n - 1 if n == 128 else 0):n, :], reads=[xn])

        def to_fm(src, n, dst, c0):
            for g in range(4):
                b = ps()
                for i in range(4):
                    kc = g * 4 + i
                    S.op("pe", lambda e, kc=kc, i=i, b=b: e.transpose(b[:, i * 128:i * 128 + n],
                                                                       src[0:n, kc * 128:(kc + 1) * 128],
                                                                       ident[0:n, 0:n]),
                         [src, ident], [b], inc=(i == 3))
                eng = "act" if g % 2 == 0 else "dve"
                o = dst[:, g * 4:(g + 1) * 4, c0:c0 + n]
                i_ = b[:, :].rearrange("p (a c) -> p a c", a=4)[:, :, 0:n]
                if eng == "act":
                    act(lambda e, o=o, i_=i_: e.activation(out=o, in_=i_, func=AF.Copy), [b], [dst])
                else:
                    dve(lambda e, o=o, i_=i_: e.tensor_copy(out=o, in_=i_), [b], [dst])

        def gemm_fm(sl, wview, c0, m, rhsT, n, nk=NKC):
            b = ps()
            for kc in range(nk):
                mm(b[0:m, 0:n], wview[:, kc, c0:c0 + m], rhsT[:, kc, 0:n], kc == 0, kc == nk - 1,
                   [sl, rhsT], [b])
            return b

        def tshift(b, m, n, mode, mucol, cl, cidx, out_ap, out_tl):
            Pb = P[p_rr[0] % 2]
            p_rr[0] += 1
            if mode == "p":
                act(lambda e: e.activation(out=Pb[0:m, 1:n + 1], in_=b[0:m, 0:n], func=AF.Copy), [b], [Pb])
                act(lambda e: e.activation(out=Pb[0:m, 0:1], in_=cl[0:m, cidx:cidx + 1], func=AF.Copy),
                    [cl, Pb], [Pb])
                act(lambda e: e.activation(out=cl[0:m, cidx:cidx + 1], in_=Pb[0:m, n:n + 1], func=AF.Copy),
                    [Pb, cl], [cl])
                t = T6()
                dve(lambda e: e.tensor_tensor(out=t[0:m, 0:n], in0=Pb[0:m, 0:n], in1=Pb[0:m, 1:n + 1],
                                              op=ALU.subtract), [Pb], [t])
                dve(lambda e: e.scalar_tensor_tensor(out=out_ap, in0=t[0:m, 0:n], scalar=par[0:m, mucol:mucol + 1],
                                                     in1=Pb[0:m, 1:n + 1], op0=ALU.mult, op1=ALU.add),
                    [t, par, Pb], [out_tl])
            else:
                act(lambda e: e.activation(out=Pb[0:m, 0:2 * n], in_=b[0:m, 0:2 * n], func=AF.Copy), [b], [Pb])
                t = T6()
                dve(lambda e: e.tensor_tensor(out=t[0:m, 0:n], in0=Pb[0:m, n:2 * n], in1=Pb[0:m, 0:n],
                                              op=ALU.subtract), [Pb], [t])
                dve(lambda e: e.scalar_tensor_tensor(out=out_ap, in0=t[0:m, 0:n], scalar=par[0:m, mucol:mucol + 1],
                                                     in1=Pb[0:m, 0:n], op0=ALU.mult, op1=ALU.add),
                    [t, par, Pb], [out_tl])

        def headnorm_fm(src_tl, src_ap, n, red, inv_cnt, eps_ap, eps_tl, out_tl, out_ap):
            b1 = ps()
            mm(b1[:, 0:n], red[:], src_ap, True, True, [red, src_tl], [b1])
            sq = T6()
            act(lambda e: e.activation(out=sq[:, 0:n], in_=src_ap, func=AF.Square), [src_tl], [sq])
            b2 = ps()
            mm(b2[:, 0:n], red[:], sq[:, 0:n], True, True, [red, sq], [b2])
            mean = T6()
            dve(lambda e: e.tensor_scalar(out=mean[:, 0:n], in0=b1[:, 0:n], scalar1=inv_cnt, scalar2=None,
                                          op0=ALU.mult), [b1], [mean])
            msq = T6()
            dve(lambda e: e.tensor_tensor(out=msq[:, 0:n], in0=mean[:, 0:n], in1=mean[:, 0:n], op=ALU.mult),
                [mean], [msq])
            var = T6()
            dve(lambda e: e.scalar_tensor_tensor(out=var[:, 0:n], in0=b2[:, 0:n], scalar=inv_cnt, in1=msq[:, 0:n],
                                                 op0=ALU.mult, op1=ALU.subtract), [b2, msq], [var])
            dve(lambda e: e.tensor_tensor(out=var[:, 0:n], in0=var[:, 0:n], in1=eps_ap, op=ALU.add),
                [var, eps_tl], [var])
            act(lambda e: e.activation(out=var[:, 0:n], in_=var[:, 0:n], func=AF.Sqrt), [var], [var])
            dve(lambda e: e.reciprocal(out=var[:, 0:n], in_=var[:, 0:n]), [var], [var])
            dve(lambda e: e.tensor_tensor(out=out_ap, in0=src_ap, in1=mean[:, 0:n], op=ALU.subtract),
                [src_tl, mean], [out_tl])
            dve(lambda e: e.tensor_tensor(out=out_ap, in0=out_ap, in1=var[:, 0:n], op=ALU.mult),
                [out_tl, var], [out_tl])

        def flat8(tl):
            return tl[:, :, :].rearrange("p a c -> p (a c)")

        DECt = RW["DEC"]
        for tl_ in (DECt, DG[0], DG[1]):
            S.op("dve", lambda e, t=tl_: e.memset(t[:], 0.0), writes=[tl_])
        S.op("dve", lambda e: e.memset(Tbd[:], 0.0), writes=[Tbd])
        Abd = (DECt, DECt[:, 0:2, :]); Rbd = (DECt, DECt[:, 2:4, :])
        Bbd = (DECt, DECt[:, 4:6, :]); Kbd = (DECt, DECt[:, 6:8, :])
        g0 = flat8(DG[0]).rearrange("p (f c t) -> p f c t", f=2, c=2)
        g1 = flat8(DG[1]).rearrange("p (f c t) -> p f c t", f=2, c=2)
        g2 = flat8(DG[2]).rearrange("p (f t) -> p f t", f=4)
        VTbd = (DG[0], g0[:, 0]); Bpbd = (DG[0], g0[:, 1]); Kpbd = (DG[1], g1[:, 0])
        Uc = [(DG[1], g1[:, 1, c]) for c in range(2)]
        st_v = [flat8(STs[c]).rearrange("p (f t) -> p f t", f=4) for c in range(2)]
        Vbd = [(STs[c], st_v[c][:, 0]) for c in range(2)]
        Bptm = [(STs[c], st_v[c][:, 1]) for c in range(2)]
        Kptm = [(STs[c], st_v[c][:, 2]) for c in range(2)]
        AKT = [(STs[c], st_v[c][:, 3]) for c in range(2)]
        ARBT = [(DG[2], g2[:, 2 * c]) for c in range(2)]
        ARKT = [(DG[2], g2[:, 2 * c + 1]) for c in range(2)]
        xt = [sb(f"cx{i}", (128, 128)) for i in range(6)]
        NTa = [(S0[c], S0[c][:, :]) for c in range(2)]
        NTb = [(S1[c], S1[c][:, :]) for c in range(2)]
        Na = [(xt[c], xt[c][:, :]) for c in range(2)]
        Nb = [(xt[2 + c], xt[2 + c][:, :]) for c in range(2)]
        Lm = [(xt[4 + c], xt[4 + c][:, :]) for c in range(2)]
        AmT = [(S0[2], S0[2][:, :]), (S1[2], S1[2][:, :])]
        Zt = [[sb(f"Z{c}_{i}", (128, 256)) for i in range(2)] for c in range(2)]
        ev_rr = [0]

        def evac(dst, src_tl, src_ap):
            ev_rr[0] += 1
            if ev_rr[0] % 2 == 0:
                act(lambda e: e.activation(out=dst[1], in_=src_ap, func=AF.Copy), [src_tl], [dst[0]])
            else:
                dve(lambda e: e.tensor_copy(out=dst[1], in_=src_ap), [src_tl], [dst[0]])

        def tr(src, dst):
            b = ps()
            S.op("pe", lambda e: e.transpose(b[:, 0:128], src[1], ident[:, :]), [src[0], ident], [b])
            evac(dst, b, b[:, 0:128])

        def amat(lhs, rhs, mi, dst):
            b = ps()
            mm(b[:, 0:128], lhs[1], rhs[1], True, True, [lhs[0], rhs[0]], [b])
            dve(lambda e: e.tensor_tensor(out=dst[1], in0=b[:, 0:128], in1=cm[:, mi, :], op=ALU.mult),
                [b, cm], [dst[0]])

        def chunk_pair(l, fb):
            Ea, Eb, Ec = qf, kf, kfr
            R, KM, V, KK, B = (RW[k] for k in ("R", "KM", "V", "KK", "B"))
            c2 = lambda ap: ap.rearrange("p (c t) -> p c t", c=2)
            bt = ps()
            S.op("pe", lambda e: e.transpose(bt[:, 0:128], SG[:, 0:128], ident[:, :]), [SG, ident], [bt])
            act(lambda e: e.activation(out=yf[:, 0:128], in_=bt[:, 0:128], func=AF.Copy), [bt], [yf])
            bc_ = ps()
            mm(bc_[:, 0:128], yf[:, 0:128], cm[:, 4, :], True, True, [yf, cm], [bc_])
            act(lambda e: e.activation(out=Ea[:, 0:128], in_=bc_[:, 0:128], func=AF.Copy), [bc_], [Ea])

            def bdw(dst, src_tl, src_ap_fn, E_tl):
                for h2 in range(2):
                    r_ = slice(h2 * 64, (h2 + 1) * 64)
                    if E_tl is None:
                        S.op("pool", lambda e, r_=r_: e.tensor_copy(out=dst[1][r_, :, r_], in_=c2(src_ap_fn(r_))),
                             [src_tl], [dst[0]])
                    else:
                        S.op("pool", lambda e, r_=r_: e.tensor_tensor(out=dst[1][r_, :, r_], in0=c2(src_ap_fn(r_)),
                                                                      in1=c2(E_tl[r_, 0:128]), op=ALU.mult),
                             [src_tl, E_tl], [dst[0]])

            dve(lambda e: e.tensor_tensor(out=Eb[:, 0:128], in0=Ea[:, 0:128], in1=SG[:, 0:128], op=ALU.subtract),
                [Ea, SG], [Eb])
            act(lambda e: e.activation(out=Eb[:, 0:128], in_=Eb[:, 0:128], func=AF.Exp, scale=-C_DEC), [Eb], [Eb])
            bdw(Abd, KK, lambda r_: KK[r_, fb, 0:128], Eb)
            act(lambda e: e.activation(out=Ec[:, 0:128], in_=Ea[:, 0:128], func=AF.Exp, scale=-C_DEC), [Ea], [Ec])
            bdw(Rbd, R, lambda r_: R[r_, fb, 0:128], Ec)
            act(lambda e: e.activation(out=Eb[:, 0:128], in_=Ea[:, 0:128], func=AF.Exp, scale=C_DEC), [Ea], [Eb])
            bdw(Bbd, B, lambda r_: B[r_, fb, 0:128], Eb)
            bdw(Kbd, KM, lambda r_: KM[r_, fb, 0:128], Eb)
            dve(lambda e: e.tensor_tensor(out=c2(Eb[:, 0:128]), in0=c2(Ea[:, 0:128])[:, :, 63:64].to_broadcast([128, 2, 64]),
                                          in1=c2(Ea[:, 0:128]), op=ALU.subtract), [Ea], [Eb])
            act(lambda e: e.activation(out=Eb[:, 0:128], in_=Eb[:, 0:128], func=AF.Exp, scale=-C_DEC), [Eb], [Eb])
            bdw(Bpbd, B, lambda r_: B[r_, fb, 0:128], Eb)
            bdw(Kpbd, KM, lambda r_: KM[r_, fb, 0:128], Eb)
            bdw(VTbd, V, lambda r_: V[r_, fb, 0:128], None)
            Zc = [0, 0]
            for c in range(2):
                fam = lambda f: (f[0], f[1][:, c, :])
                tr(fam(VTbd), Vbd[c])
                tr(fam(Abd), (Zt[c][0], Zt[c][0][:, 0:128]))
                tr(fam(Bpbd), Bptm[c])
                tr(fam(Kpbd), Kptm[c])
                amat(fam(Bbd), fam(Abd), 0, NTa[c])
                amat(fam(Abd), fam(Bbd), 1, Na[c])
                amat(fam(Kbd), fam(Abd), 2, AKT[c])
                amat(fam(Bbd), fam(Rbd), 3, ARBT[c])
                amat(fam(Kbd), fam(Rbd), 3, ARKT[c])
            for c in range(2):
                b = ps()
                mm(b[:, 0:128], AKT[c][1], Vbd[c][1], True, True, [AKT[c][0], Vbd[c][0]], [b])
                evac((Zt[c][0], Zt[c][0][:, 128:256]), b, b[:, 0:128])
            NT = [NTa, NTb]
            NN = [Na, Nb]
            for i in range(6):
                cur, nxt = i % 2, (i + 1) % 2
                for c in range(2):
                    nt, nn = NT[cur][c], NN[cur][c]
                    dve(lambda e, nt=nt, c=c: e.tensor_tensor(out=Lm[c][1], in0=nt[1], in1=ident[:, :], op=ALU.add),
                        [nt[0], ident], [Lm[c][0]])
                    zc, zn = Zt[c][i % 2], Zt[c][(i + 1) % 2]
                    b = ps()
                    mm(b[:, 0:256], Lm[c][1], zc[:, 0:256], True, True, [Lm[c][0], zc], [b])
                    evac((zn, zn[:, 0:256]), b, b[:, 0:256])
                    if i < 5:
                        b2 = ps()
                        mm(b2[:, 0:128], nn[1], nt[1], True, True, [nn[0], nt[0]], [b2])
                        evac(NT[nxt][c], b2, b2[:, 0:128])
                    if i < 4:
                        b3 = ps()
                        mm(b3[:, 0:128], nt[1], nn[1], True, True, [nt[0], nn[0]], [b3])
                        evac(NN[nxt][c], b3, b3[:, 0:128])
            Zf = [Zt[c][0] for c in range(2)]
            for c in range(2):
                tr((Zf[c], Zf[c][:, 0:128]), AmT[c])
            for h2 in range(2):
                r_ = slice(h2 * 64, (h2 + 1) * 64)
                S.op("pool", lambda e, r_=r_: e.tensor_copy(out=Tbd[r_, r_], in_=Tst[l][r_, fb, :]), [Tst[l]], [Tbd])
            for c in range(2):
                b = ps()
                mm(b[:, 0:128], AmT[c][1], Tbd[:, :], True, True, [AmT[c][0], Tbd], [b])
                dve(lambda e, b=b, c=c: e.scalar_tensor_tensor(out=Uc[c][1], in0=b[:, 0:128], scalar=-1.0,
                                                               in1=Zf[c][:, 128:256], op0=ALU.mult, op1=ALU.subtract),
                    [b, Zf[c]], [Uc[c][0]])
                bo = ps()
                mm(bo[:, 0:128], Tbd[:, :], Rbd[1][:, c, :], True, False, [Tbd, Rbd[0]], [bo])
                mm(bo[:, 0:128], Uc[c][1], ARBT[c][1], False, False, [Uc[c][0], ARBT[c][0]], [bo])
                mm(bo[:, 0:128], Vbd[c][1], ARKT[c][1], False, True, [Vbd[c][0], ARKT[c][0]], [bo])
                for h2 in range(2):
                    r_ = slice(h2 * 64, (h2 + 1) * 64)
                    evac((RW["O"], RW["O"][r_, fb, c * 64:(c + 1) * 64]), bo, bo[r_, r_])
                bs_ = ps()
                mm(bs_[:, 0:128], Bptm[c][1], Uc[c][1], True, False, [Bptm[c][0], Uc[c][0]], [bs_])
                mm(bs_[:, 0:128], Kptm[c][1], Vbd[c][1], False, True, [Kptm[c][0], Vbd[c][0]], [bs_])
                dve(lambda e, bs_=bs_, c=c: e.scalar_tensor_tensor(out=Tbd[:, :], in0=Tbd[:, :],
                                                                   scalar=Ec[:, c * 64 + 63:c * 64 + 64],
                                                                   in1=bs_[:, 0:128], op0=ALU.mult, op1=ALU.add),
                    [Tbd, Ec, bs_], [Tbd])
            for h2 in range(2):
                r_ = slice(h2 * 64, (h2 + 1) * 64)
                S.op("pool", lambda e, r_=r_: e.tensor_copy(out=Tst[l][r_, fb, :], in_=Tbd[r_, r_]), [Tbd], [Tst[l]])

        def layer(l, mode, n, tile_idx, last_tile):
            nrhs = n if mode == "p" else 2 * n
            S.dma("sp", par[:], dr["par"][l], writes=[par])
            dve(lambda e: e.tensor_scalar(out=omka[:], in0=par[:, 59:67], scalar1=-1.0, scalar2=1.0,
                                          op0=ALU.mult, op1=ALU.add), [par], [omka])
            sh_out = None
            if mode == "p" and last_tile:
                sh_out = dr["shp"][l:l + 1, :]
            if mode == "s":
                sh_out = dr["shs"][l, tile_idx * SPC:(tile_idx + 1) * SPC, :]
            norm(n, 2 * l, sh_out)
            to_fm(xn, n, xnT, 0)
            if mode == "s":
                S.dma("sp", xn[0:n, :], dr["ssh"][l, tile_idx * SPC:(tile_idx + 1) * SPC, :], reads=[], writes=[xn])
                to_fm(xn, n, xnT, n)
            for cb in range(2):
                sl = wload("win_v", (l, cb,), 512, lambda s: s[:, :].rearrange("p (k c) -> p k c", k=NKC))
                wv = sl[:, :].rearrange("p (k c) -> p k c", k=NKC)
                b = ps()
                for kc in range(NKC):
                    mm(b[0:n, :], xnT[:, kc, 0:n], wv[:, kc, :], kc == 0, kc == NKC - 1, [sl, xnT], [b])
                if mode == "p":
                    for vi, vt in ((0, v1), (1, v2)):
                        dve(lambda e, vi=vi, vt=vt, b=b, cb=cb: e.tensor_tensor(
                            out=vt[:, cb * 4:(cb + 1) * 4, :], in0=b[:, :].rearrange("p (a c) -> p a c", a=4),
                            in1=gtab[:, vi, cb * 4:(cb + 1) * 4].unsqueeze(2).to_broadcast([128, 4, 128]),
                            op=ALU.mult), [b, gtab], [vt])
                else:
                    act(lambda e, b=b, cb=cb: e.activation(out=vs[0:n, cb * 512:(cb + 1) * 512], in_=b[0:n, :],
                                                           func=AF.Copy), [b], [vs])
            for hh in range(8):
                sl = wload("win_a", (l, hh,), 384, lambda s: s[:, 0:NKC * 384].rearrange("p (k c) -> p k c", k=NKC))
                wv = sl[:, 0:NKC * 384].rearrange("p (k c) -> p k c", k=NKC)
                bq = gemm_fm(sl, wv, 0, 128, xnT, n)
                bk = gemm_fm(sl, wv, 128, 128, xnT, n)
                bg = gemm_fm(sl, wv, 256, 128, xnT, n)
                act(lambda e, bq=bq: e.activation(out=qf[:, 0:n], in_=bq[:, 0:n], func=AF.Copy), [bq], [qf])
                act(lambda e, bk=bk: e.activation(out=kf[:, 0:n], in_=bk[:, 0:n], func=AF.Copy), [bk], [kf])
                act(lambda e, bg=bg: e.activation(out=SG[:, 0:n], in_=bg[:, 0:n], func=AF.Silu), [bg], [SG])
                for src, dstf, dstb, ci in ((qf, qfr, qT, 0), (kf, kfr, kT, 2)):
                    br = ps()
                    mm(br[:, 0:n], rotT[:], src[:, 0:n], True, True, [rotT, src], [br])
                    t = T6()
                    dve(lambda e, t=t, src=src, ci=ci: e.tensor_tensor(out=t[:, 0:n], in0=src[:, 0:n],
                                                                        in1=rope[:, ci, 0:n], op=ALU.mult),
                        [src, rope], [t])
                    t2 = T6()
                    dve(lambda e, t2=t2, br=br, ci=ci: e.tensor_tensor(out=t2[:, 0:n], in0=br[:, 0:n],
                                                                        in1=rope[:, ci + 1, 0:n], op=ALU.mult),
                        [br, rope], [t2])
                    dve(lambda e, t=t, t2=t2, dstf=dstf: e.tensor_tensor(out=dstf[:, 0:n], in0=t[:, 0:n],
                                                                          in1=t2[:, 0:n], op=ALU.add),
                        [t, t2], [dstf])
                    act(lambda e, dstf=dstf, dstb=dstb: e.activation(out=dstb[:, 0:n], in_=dstf[:, 0:n],
                                                                     func=AF.Copy), [dstf], [dstb])
                by = psb[7]
                if mode == "p":
                    bt = ps()
                    S.op("pe", lambda e, bt=bt: e.transpose(bt[:, 0:128], kfr[:, 0:128], ident[:, :]),
                         [kfr, ident], [bt])
                    act(lambda e, bt=bt: e.activation(out=ktm[:, :], in_=bt[:, 0:128], func=AF.Copy), [bt], [ktm])
                    bs = ps()
                    mm(bs[:, 0:128], kT[:, 0:128], qT[:, 0:128], True, True, [kT, qT], [bs])
                    dve(lambda e, bs=bs: e.tensor_tensor(out=sTm[:, :], in0=bs[:, 0:128], in1=maskT[:, :],
                                                         op=ALU.mult), [bs, maskT], [sTm])
                    mm(by[:, 0:128], v1[:, hh, :], sTm[:, :], True, False, [v1, sTm], [by])
                    mm(by[:, 0:128], Sret[l][hh][:, :], qfr[:, 0:128], False, True, [Sret[l][hh], qfr], [by])
                    bkv = ps()
                    mm(bkv[:, 0:128], ktm[:, :], v2[:, hh, :], True, True, [ktm, v2], [bkv])
                    St = Sret[l][hh]
                    dve(lambda e, St=St, bkv=bkv, hh=hh: e.scalar_tensor_tensor(
                        out=St[:, :], in0=St[:, :], scalar=float(GAM[hh] ** 128), in1=bkv[:, 0:128],
                        op0=ALU.mult, op1=ALU.add), [St, bkv], [St])
                    if last_tile:
                        S.dma("sp", dr["retp"][l, hh], St[:, :], reads=[St])
                    et = ept[hh % 2]
                    S.dma("sp", et[:, :], dr["epst"][:, hh, :], writes=[et])
                    eps_ap, eps_tl = et[:, 0:n], et
                else:
                    bt = ps()
                    S.op("pe", lambda e, bt=bt: e.transpose(bt[0:n, 0:128], kfr[:, 0:n], ident[:, :]),
                         [kfr, ident], [bt])
                    act(lambda e, bt=bt: e.activation(out=ktm32[0:n, :], in_=bt[0:n, 0:128], func=AF.Copy),
                        [bt], [ktm32])
                    for t in range(n):
                        s0 = S0[t % 3]
                        s1 = S1[t % 3]
                        S.dma("sp", s0[:, :], dr["sret"][l, tile_idx * SPC + t, hh], writes=[s0])
                        vm = Vm[t % 2]
                        dve(lambda e, vm=vm, hh=hh, t=t: e.tensor_scalar(
                            out=vm[:, :], in0=vs[0:16, hh * 128:(hh + 1) * 128], scalar1=eye16[:, t:t + 1],
                            scalar2=None, op0=ALU.mult), [vs, eye16], [vm])
                        bkv = ps()
                        mm(bkv[:, 0:128], ktm32[0:n, :], vm[:, :], True, True, [ktm32, vm], [bkv])
                        dve(lambda e, s0=s0, s1=s1, bkv=bkv, hh=hh: e.scalar_tensor_tensor(
                            out=s1[:, :], in0=s0[:, :], scalar=float(GAM[hh]), in1=bkv[:, 0:128],
                            op0=ALU.mult, op1=ALU.add), [s0, bkv], [s1])
                        S.dma("sp", dr["rets"][l, tile_idx * SPC + t, hh], s1[:, :], reads=[s1])
                        mm(by[:, t:t + 1], s1[:, :], qfr[:, t:t + 1], True, True, [s1, qfr], [by], inc=(t == n - 1))
                    eps_ap, eps_tl = epsc[:, 0:n], epsc
                act(lambda e, by=by: e.activation(out=yf[:, 0:n], in_=by[:, 0:n], func=AF.Copy), [by], [yf])
                yn = yn_t
                headnorm_fm(yf, yf[:, 0:n], n, ones, 1.0 / 128, eps_ap, eps_tl, yn, yn[:, 0:n])
                dve(lambda e, yn=yn, hh=hh: e.scalar_tensor_tensor(
                    out=yT[:, hh, 0:n], in0=yn[:, 0:n], scalar=par[:, 91 + hh:92 + hh], in1=SG[:, 0:n],
                    op0=ALU.mult, op1=ALU.mult), [yn, par, SG], [yT])
            sl = wload("win_l", (l,), 320, lambda s: s[:, 0:NKC * 320].rearrange("p (k c) -> p k c", k=NKC))
            wv = sl[:, 0:NKC * 320].rearrange("p (k c) -> p k c", k=NKC)
            for sbk, m in ((0, 128), (1, 128), (2, 64)):
                b = gemm_fm(sl, wv, sbk * 128, m, xnT, nrhs)
                tshift(b, m, n, mode, 24 + sbk, carry[l], 24 + sbk, LX[sbk][0:m, 0:n], LX[sbk])
            act(lambda e: e.activation(out=LX[0][0:64, 0:n], in_=LX[0][0:64, 0:n], func=AF.Tanh), [LX[0]], [LX[0]])
            act(lambda e: e.activation(out=LX[1][:, 0:n], in_=LX[1][:, 0:n], func=AF.Sigmoid), [LX[1]], [LX[1]])
            act(lambda e: e.activation(out=LX[2][0:32, 0:n], in_=LX[2][0:32, 0:n], func=AF.Sigmoid), [LX[2]], [LX[2]])
            R, KM, V, DEC, KK, B, VF, O = (RW[k] for k in ("R", "KM", "V", "DEC", "KK", "B", "VF", "O"))
            for fb in range(8):
                fc = slice(0, 128)
                lup = lupb[lup_rr[0] % 2]
                lup_rr[0] += 1
                for i in ((0, 2) if l > 0 else (0,)):
                    S.dma("sp", lup[i][:, :], dr["lup"][l, i, :, fb * 128:(fb + 1) * 128], writes=[lup[i]])
                sl = wload("win_a", (l, 8 + fb,), 384, lambda s: s[:, 0:NKC * 384].rearrange("p (k c) -> p k c", k=NKC))
                wv = sl[:, 0:NKC * 384].rearrange("p (k c) -> p k c", k=NKC)
                for j, arr in ((0, R), (1, None), (2, V)):
                    b = gemm_fm(sl, wv, j * 128, 128, xnT, nrhs)
                    if arr is None:
                        kraw = kraw_t
                        tshift(b, 128, n, mode, 8 * j + fb, carry[l], 8 * j + fb, kraw[:, 0:n], kraw)
                    else:
                        tshift(b, 128, n, mode, 8 * j + fb, carry[l], 8 * j + fb, arr[:, fb, 0:n], arr)
                b = ps()
                mm(b[:, 0:n], lup[0][0:64, fc], LX[0][0:64, 0:n], True, True, [lup[0], LX[0]], [b])
                t = SG if mode == "p" else T6()
                act(lambda e, b=b, t=t, fb=fb: e.activation(out=t[:, 0:n], in_=b[:, 0:n], func=AF.Sigmoid,
                                                            bias=par[:, 27 + fb:28 + fb]), [b, par], [t])
                if mode == "s":
                    act(lambda e, t=t, fb=fb: e.activation(out=DEC[:, fb, 0:n], in_=t[:, 0:n], func=AF.Exp,
                                                           scale=-C_DEC), [t], [DEC])
                b = ps()
                mm(b[:, 0:n], lup[0][64:128, fc], LX[0][64:128, 0:n], True, True, [lup[0], LX[0]], [b])
                At = At_t
                act(lambda e, b=b, fb=fb, At=At: e.activation(out=At[:, 0:n], in_=b[:, 0:n], func=AF.Sigmoid,
                                                              bias=par[:, 35 + fb:36 + fb]), [b, par], [At])
                if l == 0:
                    act(lambda e, fb=fb: e.activation(out=VF[:, fb, 0:n], in_=V[:, fb, 0:n], func=AF.Copy), [V], [VF])
                else:
                    b = ps()
                    mm(b[:, 0:n], lup[2][32:64, fc], LX[2][32:64, 0:n], True, True, [lup[2], LX[2]], [b])
                    vg = T6()
                    act(lambda e, b=b, vg=vg, fb=fb: e.activation(out=vg[:, 0:n], in_=b[:, 0:n], func=AF.Sigmoid,
                                                                  bias=par[:, 43 + fb:44 + fb]), [b, par], [vg])
                    dd = T6()
                    dve(lambda e, dd=dd, fb=fb: e.tensor_tensor(out=dd[:, 0:n], in0=VF[:, fb, 0:n], in1=V[:, fb, 0:n],
                                                                op=ALU.subtract), [VF, V], [dd])
                    dve(lambda e, dd=dd, vg=vg: e.tensor_tensor(out=dd[:, 0:n], in0=dd[:, 0:n], in1=vg[:, 0:n],
                                                                op=ALU.mult), [dd, vg], [dd])
                    dve(lambda e, dd=dd, fb=fb: e.tensor_tensor(out=V[:, fb, 0:n], in0=V[:, fb, 0:n], in1=dd[:, 0:n],
                                                                op=ALU.add), [V, dd], [V])
                kx = T6()
                dve(lambda e, kx=kx, kraw=kraw, fb=fb: e.tensor_scalar(out=kx[:, 0:n], in0=kraw[:, 0:n],
                                                                       scalar1=par[:, 51 + fb:52 + fb], scalar2=None,
                                                                       op0=ALU.mult), [kraw, par], [kx])
                sq = T6()
                act(lambda e, sq=sq, kx=kx: e.activation(out=sq[:, 0:n], in_=kx[:, 0:n], func=AF.Square), [kx], [sq])
                b = ps()
                mm(b[:, 0:n], onesblk[:, :], sq[:, 0:n], True, True, [onesblk, sq], [b])
                rn = T6()
                dve(lambda e, rn=rn, b=b: e.tensor_scalar(out=rn[:, 0:n], in0=b[:, 0:n], scalar1=1e-24, scalar2=None,
                                                          op0=ALU.add), [b], [rn])
                act(lambda e, rn=rn: e.activation(out=rn[:, 0:n], in_=rn[:, 0:n], func=AF.Sqrt), [rn], [rn])
                dve(lambda e, rn=rn: e.reciprocal(out=rn[:, 0:n], in_=rn[:, 0:n]), [rn], [rn])
                dve(lambda e, kx=kx, rn=rn, fb=fb: e.tensor_tensor(out=KK[:, fb, 0:n], in0=kx[:, 0:n], in1=rn[:, 0:n],
                                                                   op=ALU.mult), [kx, rn], [KK])
                t = T6()
                dve(lambda e, t=t, fb=fb, At=At: e.tensor_scalar(out=t[:, 0:n], in0=At[:, 0:n],
                                                                 scalar1=par[:, 59 + fb:60 + fb], scalar2=omka[:, fb:fb + 1],
                                                                 op0=ALU.mult, op1=ALU.add), [At, par, omka], [t])
                dve(lambda e, t=t, kraw=kraw, fb=fb: e.tensor_tensor(out=KM[:, fb, 0:n], in0=kraw[:, 0:n],
                                                                     in1=t[:, 0:n], op=ALU.mult), [kraw, t], [KM])
                dve(lambda e, fb=fb, At=At: e.tensor_tensor(out=B[:, fb, 0:n], in0=KK[:, fb, 0:n], in1=At[:, 0:n],
                                                            op=ALU.mult), [KK, At], [B])
                if mode == "p":
                    chunk_pair(l, fb)
            if mode == "p" and last_tile:
                so = T6()
                for fb in range(8):
                    for h2 in range(2):
                        r_ = slice(h2 * 64, (h2 + 1) * 64)
                        S.op("pool", lambda e, r_=r_, fb=fb: e.tensor_copy(out=Tbd[r_, r_], in_=Tst[l][r_, fb, :]),
                             [Tst[l]], [Tbd])
                    bt = ps()
                    S.op("pe", lambda e, bt=bt: e.transpose(bt[:, 0:128], Tbd[:, :], ident[:, :]), [Tbd, ident], [bt])
                    for h2 in range(2):
                        r_ = slice(h2 * 64, (h2 + 1) * 64)
                        act(lambda e, r_=r_, fb=fb, bt=bt: e.activation(out=so[r_, fb * 64:(fb + 1) * 64],
                                                                      in_=bt[r_, r_], func=AF.Copy), [bt], [so])
                S.dma("sp", dr["rwp"][l].rearrange("(f h) v k -> (h v) f k", h=2),
                      so[:, :].rearrange("p (a c) -> p a c", a=8), reads=[so])
            for t in range(n if mode == "s" else 0):
                if True:
                    ST = STs[t % 2]
                    S.dma("sp", ST[:, :, :], dr["srw"][l, tile_idx * SPC + t].rearrange("(f h) v k -> (h v) f k", h=2), writes=[ST])
                bc = []
                for i, arr in enumerate((KK, DEC, B, KM, R)):
                    dg = DG[dg_rr[0] % 3]
                    dg_rr[0] += 1
                    S.op("pool", lambda e, dg=dg, arr=arr, t=t: e.tensor_tensor(
                        out=dg[:, :, :], in0=arr[:, :, t:t + 1].to_broadcast([128, 8, 64]),
                        in1=i64s[:, :].unsqueeze(1).to_broadcast([128, 8, 64]), op=ALU.mult),
                        [arr, i64s], [dg])
                    b = ps()
                    mm(b[:, :], onesblk[:, :], dg[:, :, :].rearrange("p a c -> p (a c)"), True, True,
                       [onesblk, dg], [b])
                    bc.append(b)
                bKK, bDEC, bB, bKM, bR = bc
                ta = T6(); tb = T6(); tc = T6()

                def v3(x):
                    return x[:, :].rearrange("p (a c) -> p a c", a=8)

                dve(lambda e, ta=ta, ST=ST, b=bKK: e.tensor_tensor(out=v3(ta), in0=ST[:, :, :], in1=v3(b), op=ALU.mult),
                    [ST, bKK], [ta])
                dve(lambda e, ta=ta: e.tensor_reduce(out=sa[:, :], in_=v3(ta), axis=AX.X, op=ALU.add), [ta], [sa])
                dve(lambda e, tb=tb, ST=ST, b=bDEC: e.tensor_tensor(out=v3(tb), in0=ST[:, :, :], in1=v3(b), op=ALU.mult),
                    [ST, bDEC], [tb])
                dve(lambda e, tc=tc, b=bB: e.tensor_tensor(out=v3(tc), in0=v3(b),
                                                           in1=sa[:, :].unsqueeze(2).to_broadcast([128, 8, 64]),
                                                           op=ALU.mult), [bB, sa], [tc])
                dve(lambda e, tb=tb, tc=tc: e.tensor_tensor(out=tb[:, :], in0=tb[:, :], in1=tc[:, :], op=ALU.subtract),
                    [tb, tc], [tb])
                dve(lambda e, tc=tc, b=bKM, t=t: e.tensor_tensor(out=v3(tc), in0=v3(b),
                                                                 in1=V[:, :, t:t + 1].to_broadcast([128, 8, 64]),
                                                                 op=ALU.mult), [bKM, V], [tc])
                dve(lambda e, ST=ST, tb=tb, tc=tc: e.tensor_tensor(out=ST[:, :, :], in0=v3(tb), in1=v3(tc), op=ALU.add),
                    [tb, tc], [ST])
                dve(lambda e, ta=ta, ST=ST, b=bR: e.tensor_tensor(out=v3(ta), in0=ST[:, :, :], in1=v3(b), op=ALU.mult),
                    [ST, bR], [ta])
                dve(lambda e, ta=ta, t=t: e.tensor_reduce(out=O[:, :, t:t + 1], in_=v3(ta), axis=AX.X, op=ALU.add),
                    [ta], [O])
                if mode == "s":
                    S.dma("sp", dr["rws"][l, tile_idx * SPC + t].rearrange("(f h) v k -> (h v) f k", h=2), ST[:, :, :], reads=[ST])
            for fb in range(8):
                yn = yn_t
                headnorm_fm(O, O[:, fb, 0:n], n, onesblk, 1.0 / 64, epsw[:, 0:n], epsw, yn, yn[:, 0:n])
                dve(lambda e, yn=yn, fb=fb: e.tensor_scalar(out=yn[:, 0:n], in0=yn[:, 0:n],
                                                            scalar1=par[:, 75 + fb:76 + fb],
                                                            scalar2=par[:, 83 + fb:84 + fb], op0=ALU.mult, op1=ALU.add),
                    [yn, par], [yn])
                fc = slice(0, 128)
                lup = lupb[lup_rr[0] % 2]
                lup_rr[0] += 1
                for i in (1, 2):
                    S.dma("sp", lup[i][:, :], dr["lup"][l, i, :, fb * 128:(fb + 1) * 128], writes=[lup[i]])
                t = T6()
                dve(lambda e, t=t, fb=fb: e.scalar_tensor_tensor(out=t[:, 0:n], in0=R[:, fb, 0:n],
                                                                 scalar=par[:, 67 + fb:68 + fb], in1=KM[:, fb, 0:n],
                                                                 op0=ALU.mult, op1=ALU.mult), [R, par, KM], [t])
                b = ps()
                mm(b[:, 0:n], onesblk[:, :], t[:, 0:n], True, True, [onesblk, t], [b])
                bon = T6()
                dve(lambda e, b=b, fb=fb, bon=bon: e.tensor_tensor(out=bon[:, 0:n], in0=b[:, 0:n], in1=V[:, fb, 0:n],
                                                                   op=ALU.mult), [b, V], [bon])
                dve(lambda e, yn=yn, bon=bon: e.tensor_tensor(out=yn[:, 0:n], in0=yn[:, 0:n], in1=bon[:, 0:n],
                                                              op=ALU.add), [yn, bon], [yn])
                bg = ps()
                mm(bg[:, 0:n], lup[1][:, fc], LX[1][:, 0:n], True, False, [lup[1], LX[1]], [bg])
                mm(bg[:, 0:n], lup[2][0:32, fc], LX[2][0:32, 0:n], False, True, [lup[2], LX[2]], [bg])
                dve(lambda e, yn=yn, fb=fb, bg=bg: e.tensor_tensor(out=yT[:, 8 + fb, 0:n], in0=yn[:, 0:n], in1=bg[:, 0:n],
                                                                   op=ALU.mult), [yn, bg], [yT])
            for cb in range(4):
                sl = wload("wout", (l, cb,), 512, lambda s: s[:, :].rearrange("p (k c) -> p k c", k=NKC))
                wv = sl[:, :].rearrange("p (k c) -> p k c", k=NKC)
                b = ps()
                for kc in range(NKC):
                    mm(b[0:n, :], yT[:, kc, 0:n], wv[:, kc, :], kc == 0, kc == NKC - 1, [sl, yT], [b])
                dve(lambda e, b=b, cb=cb: e.tensor_tensor(out=h[0:n, cb * 512:(cb + 1) * 512],
                                                          in0=h[0:n, cb * 512:(cb + 1) * 512], in1=b[0:n, :],
                                                          op=ALU.add), [h, b], [h])
            norm(n, 2 * l + 1)
            to_fm(xn, n, xnT, 0)
            for g in range(NG):
                slg = wload("wg", (l, g,), 512, lambda s: s[:, :].rearrange("p (k c) -> p k c", k=NKC))
                slu = wload("wu", (l, g,), 512, lambda s: s[:, :].rearrange("p (k c) -> p k c", k=NKC))
                sld = wload("wd", (l, g,), 512, lambda s: s[:, :].rearrange("p (k c) -> p k c", k=4))
                wgv = slg[:, :].rearrange("p (k c) -> p k c", k=NKC)
                wuv = slu[:, :].rearrange("p (k c) -> p k c", k=NKC)
                wdv = sld[:, :].rearrange("p (k c) -> p k c", k=4)
                aT = actT[g % 2]
                for j in range(4):
                    b1 = gemm_fm(slg, wgv, j * 128, 128, xnT, n)
                    b2 = gemm_fm(slu, wuv, j * 128, 128, xnT, n)
                    act(lambda e, b1=b1: e.activation(out=sgt[:, 0:n], in_=b1[:, 0:n], func=AF.Silu), [b1], [sgt])
                    dve(lambda e, b2=b2, j=j, aT=aT: e.tensor_tensor(out=aT[:, j, 0:n], in0=sgt[:, 0:n],
                                                                      in1=b2[:, 0:n], op=ALU.mult), [sgt, b2], [aT])
                for cb in range(4):
                    b = ps()
                    for kc in range(4):
                        mm(b[0:n, :], aT[:, kc, 0:n], wdv[:, kc, cb * 512:(cb + 1) * 512], kc == 0, kc == 3,
                           [sld, aT], [b])
                    dve(lambda e, b=b, cb=cb: e.tensor_tensor(out=h[0:n, cb * 512:(cb + 1) * 512],
                                                              in0=h[0:n, cb * 512:(cb + 1) * 512], in1=b[0:n, :],
                                                              op=ALU.add), [h, b], [h])

        for ti in range(NT):
            first_pass[0] = (ti == 0)
            S.dma("sp", h[:, :], dr["xp"][ti * TT:(ti + 1) * TT, :], writes=[h])
            S.dma("sp", rope[:, :, :], dr["rope_p"][:, :, ti * TT:(ti + 1) * TT], writes=[rope])
            for l in range(L):
                layer(l, "p", TT, ti, ti == NT - 1)
            norm(TT, 2 * L)
            S.dma("sp", dr["yp"][ti * TT:(ti + 1) * TT, :], xn[:, :], reads=[xn])
        first_pass[0] = False
        if with_sample:
            for sg in range(NSG):
                S.dma("sp", h[0:SPC, :], dr["xs"][sg * SPC:(sg + 1) * SPC, :], writes=[h])
                S.dma("sp", rope[:, :, 0:SPC], dr["rope_s"], writes=[rope])
                for l in range(L):
                    layer(l, "s", SPC, sg, False)
                norm(SPC, 2 * L)
                S.dma("sp", dr["ys"][sg * SPC:(sg + 1) * SPC, :], xn[0:SPC, :], reads=[xn])
        S.finish()

        with nc.Block() as block:
            @block.tensor
            def _(e):
                for f in S.E["pe"].q:
                    f(e)

            @block.scalar
            def _(e):
                for f in S.E["act"].q:
                    f(e)

            @block.vector
            def _(e):
                for f in S.E["dve"].q:
                    f(e)

            @block.gpsimd
            def _(e):
                for f in S.E["pool"].q:
                    f(e)

            @block.sync
            def _(e):
                for f in S.E["sp"].q:
                    f(e)
    return nc


def run(inp, L, T, B, with_sample=True):
    import time as _t
    _t0 = _t.time()
    hp = host_prep(inp, L)
    cs = host_consts(T)
    print("host_prep s", _t.time() - _t0, flush=True)
    _t0 = _t.time()
    nc = build(L, T, with_sample)
    print("build s", _t.time() - _t0, flush=True)
    _t0 = _t.time()
    in_maps = []
    for c in range(NCORE):
        m = dict(hp)
        m.update(cs)
        b = c % B
        m["xp"] = np.ascontiguousarray(inp["x_prompt"][b, :T])
        m["xs"] = np.ascontiguousarray(inp["x_sample"][c * SPCORE:(c + 1) * SPCORE, 0])
        m["sret"] = np.ascontiguousarray(inp["state_ret"][:L, c * SPCORE:(c + 1) * SPCORE])
        m["srw"] = np.ascontiguousarray(inp["state_rwkv"][:L, c * SPCORE:(c + 1) * SPCORE])
        m["ssh"] = np.ascontiguousarray(inp["state_shift"][:L, c * SPCORE:(c + 1) * SPCORE])
        in_maps.append(m)
    res = run_bass_kernel_spmd(nc, in_maps, core_ids=list(range(NCORE)))
    print("spmd s", _t.time() - _t0, flush=True)
    r = res.results
    yp = np.stack([r[b]["yp"] for b in range(B)])
    ys = np.concatenate([r[c]["ys"] for c in range(NCORE)])[:, None, :]
    retp = np.stack([r[b]["retp"] for b in range(B)], 1)
    rwp = np.stack([r[b]["rwp"] for b in range(B)], 1)
    shp = np.stack([r[b]["shp"] for b in range(B)], 1)
    rets = np.concatenate([r[c]["rets"] for c in range(NCORE)], 1)
    rws = np.concatenate([r[c]["rws"] for c in range(NCORE)], 1)
    shs = np.concatenate([r[c]["shs"] for c in range(NCORE)], 1)
    return (yp, ys, retp, rwp, shp, rets, rws, shs)


def kernel(**inputs):
    inp = {k: np.asarray(v) for k, v in inputs.items()}
    L = inp["w_in"].shape[0]
    B, T = inp["x_prompt"].shape[0], inp["x_prompt"].shape[1]
    return run(inp, L, T, B)
```
